# Optimizing a Trainium2 kernel written in Bass

```python
import math
import jax, jax.numpy as jnp
from jax import lax
import numpy as np


D_MODEL = 2048
BATCH = 1
SEQ = 8192
DEPTH = 4

N_MIXERS = 3
D_FF = 5632
FFN_RES = 0.5
NORM_EPS = 1e-6
NEG = -1e30
N_MOD = 9

MOBA_HEADS = 16
MOBA_HEAD_DIM = D_MODEL // MOBA_HEADS
MOBA_BLOCK = 256
MOBA_TOPK = 3
MOBA_QCHUNK = 64

POOL_WINDOWS = (2, 4, 8, 16)
POOL_GROUP = D_MODEL // len(POOL_WINDOWS)

SWA_HEAD_DIM = 64
SWA_Q_HEADS = D_MODEL // SWA_HEAD_DIM
SWA_KV_HEADS = SWA_Q_HEADS // 8
SWA_WINDOW = 128

N_A = (DEPTH + 2) // 3
N_B = (DEPTH + 1) // 3
N_C = DEPTH // 3

kernel_name = "hybrid_moba_pool_swa_macaron_adaln"


def rms_norm(x, g):
    xf = x.astype(jnp.float32)
    y = xf * lax.rsqrt(jnp.mean(xf * xf, axis=-1, keepdims=True) + NORM_EPS)
    return (y * g.astype(jnp.float32)).astype(x.dtype)


def modulate(h, shift, scale):
    return h * (1.0 + scale[:, None, :]) + shift[:, None, :]


def alibi_slopes(n):
    return jnp.asarray([2.0 ** (-8.0 * (i + 1) / n) for i in range(n)], jnp.float32)


def swiglu(h, w_gate, w_up, w_down):
    return (jax.nn.silu(h @ w_gate) * (h @ w_up)) @ w_down


def moba_attention(h, w_qkv, w_o):
    B, T, _ = h.shape
    H, dh, BS, QC = MOBA_HEADS, MOBA_HEAD_DIM, MOBA_BLOCK, MOBA_QCHUNK
    q, k, v = jnp.split(h @ w_qkv, 3, axis=-1)
    Tp = -(-T // BS) * BS
    pad = Tp - T

    def heads(a):
        a = jnp.pad(a, ((0, 0), (0, pad), (0, 0)))
        return a.reshape(B, Tp, H, dh).transpose(0, 2, 1, 3)

    q, k, v = heads(q), heads(k), heads(v)
    NB = Tp // BS
    kb = k.reshape(B, H, NB, BS, dh)
    vb = v.reshape(B, H, NB, BS, dh)
    kmean = jnp.mean(kb.astype(jnp.float32), axis=3)
    gate = jnp.einsum('bhtd,bhnd->bhtn', q.astype(jnp.float32), kmean)
    qblk = jnp.arange(Tp) // BS
    past = jnp.arange(NB)[None, :] < qblk[:, None]
    gate = jnp.where(past, gate, NEG)
    kk = min(MOBA_TOPK, NB)
    _, sel = lax.top_k(gate, kk)
    sel_valid = sel < qblk[None, None, :, None]
    slopes = alibi_slopes(H)
    scale = dh ** -0.5
    b_ix = jnp.arange(B)[:, None, None, None]
    h_ix = jnp.arange(H)[None, :, None, None]

    def chunk(ci):
        t0 = ci * QC
        qc = lax.dynamic_slice_in_dim(q, t0, QC, axis=2)
        sc = lax.dynamic_slice_in_dim(sel, t0, QC, axis=2)
        vc = lax.dynamic_slice_in_dim(sel_valid, t0, QC, axis=2)
        tpos = t0 + jnp.arange(QC)
        ksel = kb[b_ix, h_ix, sc]
        vsel = vb[b_ix, h_ix, sc]
        spos = sc[..., None] * BS + jnp.arange(BS)
        s_sel = jnp.einsum('bhqd,bhqnkd->bhqnk', qc, ksel,
                           preferred_element_type=jnp.float32) * scale
        s_sel = s_sel - slopes[None, :, None, None, None] * (
            tpos[None, None, :, None, None] - spos).astype(jnp.float32)
        s_sel = jnp.where(vc[..., None], s_sel, NEG)
        ob = t0 // BS
        kown = lax.dynamic_slice_in_dim(kb, ob, 1, axis=2)[:, :, 0]
        vown = lax.dynamic_slice_in_dim(vb, ob, 1, axis=2)[:, :, 0]
        dist = tpos[:, None] - (ob * BS + jnp.arange(BS))[None, :]
        s_own = jnp.einsum('bhqd,bhkd->bhqk', qc, kown,
                           preferred_element_type=jnp.float32) * scale
        s_own = s_own - slopes[None, :, None, None] * dist.astype(jnp.float32)
        s_own = jnp.where(dist >= 0, s_own, NEG)
        s_all = jnp.concatenate([s_sel.reshape(B, H, QC, kk * BS), s_own], axis=-1)
        p = jax.nn.softmax(s_all, axis=-1).astype(v.dtype)
        p_sel = p[..., :kk * BS].reshape(B, H, QC, kk, BS)
        p_own = p[..., kk * BS:]
        return (jnp.einsum('bhqnk,bhqnkd->bhqd', p_sel, vsel)
                + jnp.einsum('bhqk,bhkd->bhqd', p_own, vown))

    outs = lax.map(chunk, jnp.arange(Tp // QC))
    o = outs.transpose(1, 0, 3, 2, 4).reshape(B, Tp, H * dh)[:, :T]
    return o @ w_o


def pool_mixer(h, w_pool, pool_scale):
    B, T, D = h.shape
    G = POOL_GROUP
    hf = h.astype(jnp.float32)
    cs = jnp.concatenate([jnp.zeros((B, 1, D), jnp.float32), lax.cumsum(hf, axis=1)], axis=1)
    t = jnp.arange(T)
    outs = []
    for gi, w in enumerate(POOL_WINDOWS):
        csg = cs[..., gi * G:(gi + 1) * G]
        lo = jnp.concatenate([jnp.zeros((B, w - 1, G), jnp.float32), csg[:, :T - w + 1]], axis=1)
        cnt = jnp.minimum(t + 1, w).astype(jnp.float32)
        outs.append((csg[:, 1:] - lo) / cnt[None, :, None] - hf[..., gi * G:(gi + 1) * G])
    pooled = jnp.stack(outs, axis=2).astype(h.dtype)
    mixed = jnp.einsum('btgi,gio->btgo', pooled, w_pool).reshape(B, T, D)
    return mixed * pool_scale


def swa_sink_attention(h, w_qkv, w_o, sinks):
    B, T, _ = h.shape
    Hq, Hkv, dh, W = SWA_Q_HEADS, SWA_KV_HEADS, SWA_HEAD_DIM, SWA_WINDOW
    G = Hq // Hkv
    NBq = T // W
    qkv = h @ w_qkv
    q = qkv[..., :Hq * dh].reshape(B, NBq, W, Hkv, G, dh)
    k = qkv[..., Hq * dh:(Hq + Hkv) * dh].reshape(B, NBq, W, Hkv, dh)
    v = qkv[..., (Hq + Hkv) * dh:].reshape(B, NBq, W, Hkv, dh)

    def band(a):
        prev = jnp.pad(a, ((0, 0), (1, 0), (0, 0), (0, 0), (0, 0)))[:, :-1]
        return jnp.concatenate([prev, a], axis=2)

    kband, vband = band(k), band(v)
    s = jnp.einsum('bnqkgd,bnskd->bkgnqs', q, kband,
                   preferred_element_type=jnp.float32) * (dh ** -0.5)
    dist = (jnp.arange(W)[:, None] + W) - jnp.arange(2 * W)[None, :]
    kpos = jnp.arange(NBq)[:, None] * W - W + jnp.arange(2 * W)[None, :]
    valid = ((dist >= 0) & (dist < W))[None] & (kpos >= 0)[:, None, :]
    slopes = alibi_slopes(Hq).reshape(Hkv, G)
    s = s - slopes[None, :, :, None, None, None] * dist.astype(jnp.float32)
    s = jnp.where(valid, s, NEG)
    sink = jnp.broadcast_to(sinks.astype(jnp.float32).reshape(1, Hkv, G, 1, 1, 1), s.shape[:-1] + (1,))
    p = jax.nn.softmax(jnp.concatenate([s, sink], axis=-1), axis=-1)[..., :-1]
    o = jnp.einsum('bkgnqs,bnskd->bnqkgd', p.astype(h.dtype), vband).reshape(B, T, Hq * dh)
    return o @ w_o


def setup_inputs(seed: int = 0) -> dict:
    key = jax.random.key(seed)
    ks = jax.random.split(key, 16)
    D, F, G = D_MODEL, D_FF, POOL_GROUP
    swa_qkv_out = (SWA_Q_HEADS + 2 * SWA_KV_HEADS) * SWA_HEAD_DIM
    nrm = jax.random.normal
    return {
        'x': nrm(ks[0], (BATCH, SEQ, D), jnp.float32),
        'c': nrm(ks[1], (BATCH, D), jnp.float32),
        'norm_g': 1.0 + 0.02 * nrm(ks[2], (DEPTH, 3, D), jnp.float32),
        'ada_w': nrm(ks[3], (DEPTH, D, N_MOD * D), jnp.float32) * (0.5 * D ** -0.5),
        'ada_b': 0.01 * nrm(ks[4], (DEPTH, N_MOD * D), jnp.float32),
        'ffn_w_gate': nrm(ks[5], (DEPTH, 2, D, F), jnp.float32) * D ** -0.5,
        'ffn_w_up': nrm(ks[6], (DEPTH, 2, D, F), jnp.float32) * D ** -0.5,
        'ffn_w_down': nrm(ks[7], (DEPTH, 2, F, D), jnp.float32) * F ** -0.5,
        'moba_w_qkv': nrm(ks[8], (N_A, D, 3 * D), jnp.float32) * D ** -0.5,
        'moba_w_o': nrm(ks[9], (N_A, D, D), jnp.float32) * D ** -0.5,
        'pool_w': nrm(ks[10], (N_B, len(POOL_WINDOWS), G, G), jnp.float32) * G ** -0.5,
        'pool_scale': 1.0 + 0.02 * nrm(ks[11], (N_B, D), jnp.float32),
        'swa_w_qkv': nrm(ks[12], (N_C, D, swa_qkv_out), jnp.float32) * D ** -0.5,
        'swa_w_o': nrm(ks[13], (N_C, SWA_Q_HEADS * SWA_HEAD_DIM, D), jnp.float32) * (SWA_Q_HEADS * SWA_HEAD_DIM) ** -0.5,
        'swa_sinks': 0.5 * nrm(ks[14], (N_C, SWA_Q_HEADS), jnp.float32),
        'final_g': 1.0 + 0.02 * nrm(ks[15], (D,), jnp.float32),
    }


def reference(x, c, norm_g, ada_w, ada_b, ffn_w_gate, ffn_w_up, ffn_w_down,
              moba_w_qkv, moba_w_o, pool_w, pool_scale,
              swa_w_qkv, swa_w_o, swa_sinks, final_g):
    cs = jax.nn.silu(c)
    for i in range(DEPTH):
        mod = cs @ ada_w[i] + ada_b[i]
        sh1, sc1, g1, sh2, sc2, g2, sh3, sc3, g3 = jnp.split(mod, N_MOD, axis=-1)
        h = modulate(rms_norm(x, norm_g[i, 0]), sh1, sc1)
        x = x + FFN_RES * g1[:, None, :] * swiglu(h, ffn_w_gate[i, 0], ffn_w_up[i, 0], ffn_w_down[i, 0])
        h = modulate(rms_norm(x, norm_g[i, 1]), sh2, sc2)
        kind, j = i % N_MIXERS, i // N_MIXERS
        if kind == 0:
            y = moba_attention(h, moba_w_qkv[j], moba_w_o[j])
        elif kind == 1:
            y = pool_mixer(h, pool_w[j], pool_scale[j])
        else:
            y = swa_sink_attention(h, swa_w_qkv[j], swa_w_o[j], swa_sinks[j])
        x = x + g2[:, None, :] * y
        h = modulate(rms_norm(x, norm_g[i, 2]), sh3, sc3)
        x = x + FFN_RES * g3[:, None, :] * swiglu(h, ffn_w_gate[i, 1], ffn_w_up[i, 1], ffn_w_down[i, 1])
    return rms_norm(x, final_g)
```

```python
import numpy as np
import ml_dtypes
from contextlib import ExitStack

import concourse.bass as bass
import concourse.mybir as mybir
from concourse.bass_utils import run_bass_kernel_spmd

F32 = mybir.dt.float32
BF16 = mybir.dt.bfloat16
AF = mybir.ActivationFunctionType
ALU = mybir.AluOpType
AX = mybir.AxisListType

NCORES = 8
D = 2048
T = 8192
TC = T // NCORES
NJ = D // 128
FF = 5632
NFC = FF // 128
DEPTH = 4
EPS = 1e-6
NMODC = 9 * NJ
MODPC = DEPTH * NMODC // NCORES
BIG = 339411.0

ENGS = ('pe', 'act', 'dve', 'pool', 'sp')
SYNC_SAME = {'pe': False, 'act': True, 'dve': True, 'pool': True, 'sp': False}


class _Op:
    __slots__ = ('eng', 'fn', 'deps', 'dma', 'marked', 'val')

    def __init__(self, eng, fn, deps, dma=None):
        self.eng = eng
        self.fn = fn
        self.deps = deps
        self.dma = dma
        self.marked = False
        self.val = 0


class Sched:
    def __init__(self, n_dma_sems):
        self.ops = {e: [] for e in ENGS}
        self.lastw = {}
        self.readers = {}
        self.nd = n_dma_sems
        self.dcnt = [0] * n_dma_sems
        self.drr = 0
        self.outstanding = []

    def _deps(self, reads, writes):
        deps = []
        for r in reads:
            t = self.lastw.get(r)
            if t is not None:
                deps.append(t)
        for w in writes:
            t = self.lastw.get(w)
            if t is not None:
                deps.append(t)
            deps.extend(self.readers.get(w, ()))
        return deps

    def _commit(self, tok, reads, writes):
        for r in reads:
            lst = self.readers.setdefault(r, [])
            if tok[0] == 'e':
                for i, t in enumerate(lst):
                    if t[0] == 'e' and t[1] == tok[1]:
                        lst[i] = tok
                        break
                else:
                    lst.append(tok)
            else:
                lst.append(tok)
        for w in writes:
            self.lastw[w] = tok
            self.readers[w] = []

    def op(self, eng, fn, reads=(), writes=()):
        deps = self._deps(reads, writes)
        idx = len(self.ops[eng])
        self.ops[eng].append(_Op(eng, fn, deps))
        self._commit(('e', eng, idx), reads, writes)

    def dma(self, eng, fn, reads=(), writes=(), inc=16):
        k = self.drr
        self.drr = (k + 1) % self.nd
        prev = self.dcnt[k]
        self.dcnt[k] += inc
        val = self.dcnt[k]
        deps = self._deps(reads, writes)
        if prev > 0:
            deps.append(('d', k, prev))
        self.ops[eng].append(_Op(eng, fn, deps, dma=(k, val, inc)))
        tok = ('d', k, val)
        self.outstanding.append(tok)
        self._commit(tok, reads, writes)

    def barrier(self):
        toks = []
        for e in ENGS:
            for i in range(len(self.ops[e]) - 1, -1, -1):
                if self.ops[e][i].dma is None:
                    toks.append(('e', e, i))
                    break
        toks += self.outstanding
        for e in ENGS:
            self.ops[e].append(_Op(e, None, list(toks)))
        self.outstanding = []
        self.lastw = {}
        self.readers = {}

    def emit(self, block, sems, dsems):
        for e in ENGS:
            for o in self.ops[e]:
                for t in o.deps:
                    if t[0] == 'e' and (t[1] != e or SYNC_SAME[e]):
                        self.ops[t[1]][t[2]].marked = True
        for e in ENGS:
            c = 0
            for o in self.ops[e]:
                if o.marked:
                    c += 1
                o.val = c
        handles = {'pe': block.tensor, 'act': block.scalar, 'dve': block.vector,
                   'pool': block.gpsimd, 'sp': block.sync}
        for e in ENGS:
            ops = self.ops[e]
            if not ops:
                continue

            def body(h, e=e, ops=ops):
                waited_e = {}
                waited_d = {}
                for o in ops:
                    need_e = {}
                    need_d = {}
                    for t in o.deps:
                        if t[0] == 'e':
                            if t[1] == e and not SYNC_SAME[e]:
                                continue
                            if t[2] > need_e.get(t[1], -1):
                                need_e[t[1]] = t[2]
                        else:
                            if t[2] > need_d.get(t[1], 0):
                                need_d[t[1]] = t[2]
                    for e2, i2 in need_e.items():
                        if i2 > waited_e.get(e2, -1):
                            h.wait_ge(sems[e2], self.ops[e2][i2].val)
                            waited_e[e2] = i2
                    for k, v in need_d.items():
                        if v > waited_d.get(k, 0):
                            h.wait_ge(dsems[k], v)
                            waited_d[k] = v
                    if o.fn is None:
                        ins = h.nop() if o.marked else None
                    else:
                        ins = o.fn(h)
                    if o.dma is not None:
                        ins.then_inc(dsems[o.dma[0]], o.dma[2])
                    elif o.marked:
                        ins.then_inc(sems[e], 1)
            handles[e](body)


class Carver:
    def __init__(self, region, n):
        self.r = region
        self.n = n
        self.off = 0

    def f32(self, n):
        a = self.r[:, self.off:self.off + n]
        self.off += n
        assert self.off <= self.n, (self.off, self.n)
        return a

    def bf16(self, n):
        m = (n + 1) // 2
        a = self.r[:, self.off:self.off + m].bitcast(BF16)
        self.off += m
        assert self.off <= self.n, (self.off, self.n)
        return a

    def mark(self):
        return self.off

    def reset(self, off):
        self.off = off


INSPEC = {
    'x_in': ("x_c", [TC, D], F32),
    'cvec': ("cvec", [NJ, 128], F32),
    'normg': ("norm_g", [DEPTH * 3 * NJ, 128], F32),
    'finalg': ("final_g", [NJ, 128], F32),
    'pscale': ("pool_scale", [NJ, 128], F32),
    'adaw': ("ada_w_c", [D, MODPC * 128], F32),
    'adab': ("ada_b_c", [MODPC, 128], F32),
    'ident_in': ("ident", [128, 128], F32),
    'onehot_in': ("onehot_prev", [128, NCORES], F32),
    'pinvc_in': ("pool_invc", [128, 64], F32),
    'pool_w': ("pool_w", [4, 512, 512], F32),
    'swa_qkv': ("swa_w_qkv", [D, 2560], F32),
    'swa_o': ("swa_w_o", [D, D], F32),
    'swa_tab': ("swa_tab", [128, 96], F32),
    'swa_slq': ("swa_slq", [3, 32, 128], BF16),
    'swa_cm': ("swa_cm", [128, 3, 128], BF16),
    'moba_pastb': ("moba_pastb", [128, 128], F32),
    'moba_TL': ("moba_TL", [35, 32 * 128], BF16),
    'moba_slq': ("moba_slq", [16, 3, 2, 512], BF16),
    'moba_bias': ("moba_bias", [16, 128, 128], F32),
    'moba_cmask': ("moba_cmask", [128, 2, 256], BF16),
    'moba_bown': ("moba_bown", [128, 32], F32),
}
NTOT = 16384 + 8192 + 576 + 192 + 16 + 16 + 128 + 64 + 64 + 16 + 16 + 16 + 2 + 8 + 64 + 26000


class K:
    def __init__(self, nc, cfg):
        self.__dict__['_decl'] = {}
        self.nc = nc
        self.cfg = cfg
        self.fused = cfg.get('fused', False)
        self.used_inputs = []
        self.outputs = []

    def din(self, name, shape, dt=F32):
        if name not in self._decl:
            self._decl[name] = self.nc.dram_tensor(name, list(shape), dt, kind="ExternalInput").ap()
            self.used_inputs.append(name)
        return self._decl[name]

    def dout(self, name, shape, dt=F32):
        if name not in self._decl:
            self._decl[name] = self.nc.dram_tensor(name, list(shape), dt, kind="ExternalOutput").ap()
            self.outputs.append(name)
        return self._decl[name]

    def dint(self, name, shape, dt=F32):
        if name not in self._decl:
            self._decl[name] = self.nc.dram_tensor(name, list(shape), dt, kind="Internal").ap()
        return self._decl[name]

    def __getattr__(self, name):
        if name in INSPEC:
            nm, shp, dt = INSPEC[name]
            return self.din(nm, shp, dt)
        raise AttributeError(name)

    def ffnw(self, which, l, s):
        shp = [FF, D] if which == 'd' else [D, FF]
        return self.din("w%s_%d_%d" % (which, l, s), shp)

    def mobaw(self, which, jm):
        return self.din("moba_%s_%d" % (which, jm), [D, 3 * D] if which == 'qkv' else [D, D])

    def ex_w(self, tag, width):
        if self.fused:
            return self.dint("loc_" + tag, [128, width])
        return self.dout("ex_out", [128, width])

    def ex_all(self, tag, width):
        if self.fused:
            return self.dint("all_" + tag, [NCORES * 128, width])
        return self.din("ex_all", [NCORES * 128, width])

    def ex_own(self, tag, width):
        if self.fused:
            return self.dint("loc_" + tag, [128, width])
        return self.din("ex_own", [128, width])

    def kv_w(self, hh):
        if self.fused:
            t = self.dint("loc_kv%d" % hh, [128, 1028])
            return t[:, 0:512], t[:, 512:1028]
        t = self.ex_w('kv', KVW)
        return t[:, hh * 512:(hh + 1) * 512], t[:, 8192 + hh * 516:8192 + (hh + 1) * 516]

    def kv_own(self, hh):
        if self.fused:
            return self.kv_w(hh)
        t = self.ex_own('kv', KVW)
        return t[:, hh * 512:(hh + 1) * 512], t[:, 8192 + hh * 516:8192 + (hh + 1) * 516]

    def kv_allh(self, hh):
        if self.fused:
            t = self.dint("all_kv%d" % hh, [NCORES * 128, 1028]).rearrange("(r p) c -> p r c", p=128)
            return t[:, :, 0:512], t[:, :, 512:1028]
        t = self.ex_all('kv', KVW).rearrange("(r p) c -> p r c", p=128)
        return t[:, :, hh * 512:(hh + 1) * 512], t[:, :, 8192 + hh * 516:8192 + (hh + 1) * 516]

    def km_w(self):
        if self.fused:
            return self.dint("loc_km", [128, 64])
        return self.ex_w('kv', KVW)[:, 16448:16512]

    def km_all(self):
        if self.fused:
            return self.dint("all_km", [NCORES * 128, 64]).rearrange("(r p) c -> p r c", p=128)
        return self.ex_all('kv', KVW).rearrange("(r p) c -> p r c", p=128)[:, :, 16448:16512]

    def exchange(self, tag, width, reads, outkey):
        if not self.fused:
            return
        src = self.ex_w(tag, width)
        dst = self.ex_all(tag, width)
        self.S.dma('pool', lambda h: h.collective_compute(
            "AllGather", ALU.bypass, replica_groups=[list(range(NCORES))],
            ins=[src.opt()], outs=[dst.opt()]), reads=reads, writes=[outkey], inc=1)


SEGMENTS = [
    [('pro1',)],
    [('pro2',), ('ffn', 0, 0), ('mobaA', 0)],
    [('mobaB', 0), ('ffn', 0, 1), ('ffn', 1, 0), ('poolA', 1)],
    [('poolB', 1), ('ffn', 1, 1), ('ffn', 2, 0), ('swaA', 2)],
    [('swaB', 2), ('ffn', 2, 1), ('ffn', 3, 0), ('mobaA', 3)],
    [('mobaB', 3), ('ffn', 3, 1), ('final',)],
]


def build_program(cfg):
    seg = cfg.get('seg', None)
    nc = bass.Bass("TRN2", target_bir_lowering=False, num_devices=NCORES)
    k = K(nc, cfg)
    if seg is None:
        stages = cfg.get('stages') or [st for sg in SEGMENTS for st in sg]
        first, last = True, True
    else:
        stages = SEGMENTS[seg]
        first, last = seg == 0, seg == len(SEGMENTS) - 1
    dbg_x = cfg.get('dbg_x', False)
    with ExitStack() as es:
        SB = es.enter_context(nc.sbuf_tensor("SB", [128, NTOT], F32))
        k.SB = SB
        st = {'o': 0}

        def cv(n):
            a_ = SB[:, st['o']:st['o'] + n]
            st['o'] += n
            return a_
        k.xT = cv(NJ * TC).rearrange("p (j t) -> p j t", j=NJ)
        k.hT = cv(NJ * TC // 2).bitcast(BF16).rearrange("p (j t) -> p j t", j=NJ)
        k.modT = cv(DEPTH * NMODC)
        k.gT = cv(DEPTH * 3 * NJ)
        k.fgT = cv(NJ)
        k.psT = cv(NJ)
        k.ident32 = cv(128)
        k.ident16 = cv(64).bitcast(BF16)
        k.ones16 = cv(64).bitcast(BF16)
        k.colA = cv(NJ)
        k.colG = cv(NJ)
        k.cs32 = cv(NJ)
        k.epsc = cv(2)
        k.onehot = cv(NCORES)
        k.pinvc = cv(64)
        RN = 26000
        k.RN = RN
        k.R = cv(RN)
        assert st['o'] == NTOT
        k.PS = es.enter_context(nc.psum_tensor("PS", [128, 8 * 512], F32))
        sems = {e: es.enter_context(nc.semaphore("s_" + e)) for e in ENGS}
        ND = 24
        dsems = [es.enter_context(nc.semaphore("d%d" % i)) for i in range(ND)]
        block = es.enter_context(nc.Block())
        S = Sched(ND)
        k.S = S
        CH = 8192
        nch = (NTOT + CH - 1) // CH
        if not first:
            sin = k.din("state_in", [128, NTOT])
            for i in range(nch):
                lo, hi = i * CH, min(NTOT, (i + 1) * CH)
                S.dma('sp', lambda h, lo=lo, hi=hi: h.dma_start(out=SB[:, lo:hi], in_=sin[:, lo:hi]), writes=[('st', i)])
            S.barrier()
        for stg in stages:
            nm = stg[0]
            if nm == 'pro1':
                prologue1(k)
            elif nm == 'pro2':
                prologue2(k)
            elif nm == 'ffn':
                norm_mod(k, stg[1], 0 if stg[2] == 0 else 2)
                ffn(k, stg[1], stg[2])
            elif nm == 'mobaA':
                moba_A(k, stg[1], cfg.get('jm', stg[1] // 3))
            elif nm == 'mobaB':
                moba_B(k, stg[1], cfg.get('jm', stg[1] // 3))
            elif nm == 'poolA':
                pool_A(k, stg[1])
            elif nm == 'poolB':
                pool_B(k, stg[1])
            elif nm == 'swaA':
                swa_A(k, stg[1])
            elif nm == 'swaB':
                swa_B(k, stg[1])
            elif nm == 'final':
                S.barrier()
                if dbg_x:
                    xo = k.dout("xT_out", [128, NJ * TC])
                    S.dma('sp', lambda h: h.dma_start(out=xo.rearrange("p (j t) -> p j t", j=NJ), in_=k.xT), writes=['out'])
                else:
                    final_norm_out(k)
        fin = ['out'] + [('out', tt) for tt in range(8)] + ['exo']
        if not last:
            S.barrier()
            sout = k.dout("state_out", [128, NTOT])
            for i in range(nch):
                lo, hi = i * CH, min(NTOT, (i + 1) * CH)
                S.dma('sp', lambda h, lo=lo, hi=hi: h.dma_start(out=sout[:, lo:hi], in_=SB[:, lo:hi]), writes=[('sto', i)])
                fin.append(('sto', i))
        S.op('sp', lambda h: h.nop(), reads=fin)
        S.emit(block, sems, dsems)
    return nc, k


def ps_bank(k, b, n=512):
    return k.PS[:, b * 512:b * 512 + n]


def xkeys(hf=None):
    if hf is None:
        return [('x', j, h) for j in range(NJ) for h in range(2)]
    return [('x', j, hf) for j in range(NJ)]


def prologue1(k):
    S = k.S
    C = Carver(k.R, k.RN)
    S.dma('sp', lambda h: h.dma_start(out=k.ident32[:], in_=k.ident_in), writes=['ident32'])
    S.op('dve', lambda h: h.tensor_copy(out=k.ident16[:], in_=k.ident32[:]), reads=['ident32'], writes=['ident16'])
    S.op('dve', lambda h: h.memset(k.ones16[:], 1.0), writes=['ones16'])
    S.op('dve', lambda h: h.memset(k.epsc[:], float(D * EPS)), writes=['epsc'])
    S.dma('sp', lambda h: h.dma_start(out=k.onehot[:], in_=k.onehot_in), writes=['onehot'])
    S.dma('sp', lambda h: h.dma_start(out=k.pinvc[:], in_=k.pinvc_in), writes=['pinvc'])

    rows = C.f32(128)
    def to_cols(src_ap, nrows, dst_ap, tag, func=None):
        S.dma('sp', lambda h: h.dma_start(out=rows[0:nrows, :], in_=src_ap), writes=['rows'])
        pst = ps_bank(k, 7, nrows)
        S.op('pe', lambda h: h.transpose(out=pst, in_=rows[0:nrows, :], identity=k.ident32[0:nrows, 0:nrows]),
             reads=['rows', 'ident32'], writes=[('psb', 7)])
        if func is None:
            S.op('dve', lambda h: h.tensor_copy(out=dst_ap, in_=pst), reads=[('psb', 7)], writes=[tag])
        else:
            S.op('act', lambda h: h.activation(out=dst_ap, in_=pst, func=func), reads=[('psb', 7)], writes=[tag])
    to_cols(k.cvec, NJ, k.cs32[:], 'cs32', AF.Silu)
    to_cols(k.normg[0:96, :], 96, k.gT[:, 0:96], 'gT0')
    to_cols(k.normg[96:192, :], 96, k.gT[:, 96:192], 'gT1')
    to_cols(k.finalg, NJ, k.fgT[:], 'fgT')
    to_cols(k.pscale, NJ, k.psT[:], 'psT')
    adabT = C.f32(MODPC)
    to_cols(k.adab, MODPC, adabT, 'adabT')

    NG = 4
    nslab = MODPC // NG
    wslab = [C.f32(NJ * NG * 128).rearrange("p (j f) -> p j f", j=NJ) for _ in range(2)]
    psm = ps_bank(k, 6, MODPC)
    for sidx in range(nslab):
        w = wslab[sidx % 2]
        src = k.adaw[:, sidx * NG * 128:(sidx + 1) * NG * 128].rearrange("(j p) f -> p j f", p=128)
        S.dma('sp', lambda h, w=w, src=src: h.dma_start(out=w, in_=src), writes=[('adaw', sidx % 2)])
        for g in range(NG):
            jj = sidx * NG + g
            for kc in range(NJ):
                S.op('pe', lambda h, w=w, g=g, jj=jj, kc=kc: h.matmul(
                    psm[:, jj:jj + 1], w[:, kc, g * 128:(g + 1) * 128], k.cs32[:, kc:kc + 1],
                    start=(kc == 0), stop=(kc == NJ - 1)),
                    reads=[('adaw', sidx % 2), 'cs32'], writes=[('psb', 6)])
    modloc = C.f32(MODPC)
    S.op('dve', lambda h: h.tensor_tensor(out=modloc, in0=psm, in1=adabT, op=ALU.add),
         reads=[('psb', 6), 'adabT'], writes=['modloc'])
    mloc = k.ex_w('mod', MODPC)
    S.dma('pool', lambda h: h.dma_start(out=mloc, in_=modloc), reads=['modloc'], writes=['exo'])
    k.exchange('mod', MODPC, ['exo'], 'mod_all_d')
    S.barrier()


def prologue2(k):
    S = k.S
    C = Carver(k.R, k.RN)
    mall = k.ex_all('mod', MODPC)
    S.dma('pool', lambda h: h.dma_start(out=k.modT.rearrange("p (r c) -> p r c", r=NCORES),
                                        in_=mall.rearrange("(r p) c -> p r c", p=128)),
          reads=['mod_all_d'], writes=['modT'])

    xst = [C.f32(D) for _ in range(2)]
    for tt in range(TC // 128):
        xs = xst[tt % 2]
        S.dma('sp', lambda h, xs=xs, tt=tt: h.dma_start(out=xs, in_=k.x_in[tt * 128:(tt + 1) * 128, :]),
              writes=[('xst', tt % 2)])
        for jg in range(NJ // 4):
            b = jg % 4
            pst = ps_bank(k, b).rearrange("p (a t) -> p a t", a=4)
            for a in range(4):
                j = jg * 4 + a
                S.op('pe', lambda h, xs=xs, j=j, a=a, pst=pst: h.transpose(
                    out=pst[:, a, :], in_=xs[:, j * 128:(j + 1) * 128], identity=k.ident32[:]),
                    reads=[('xst', tt % 2), 'ident32'], writes=[('psb', b)])
            dst = k.xT[:, jg * 4:(jg + 1) * 4, tt * 128:(tt + 1) * 128]
            eng = 'dve' if jg % 2 == 0 else 'act'
            if eng == 'dve':
                S.op('dve', lambda h, dst=dst, pst=pst: h.tensor_copy(out=dst, in_=pst),
                     reads=[('psb', b)], writes=[('x', j, tt // 4) for j in range(jg * 4, jg * 4 + 4)])
            else:
                S.op('act', lambda h, dst=dst, pst=pst: h.copy(out=dst, in_=pst),
                     reads=[('psb', b)], writes=[('x', j, tt // 4) for j in range(jg * 4, jg * 4 + 4)])
    S.barrier()


def norm_mod(k, l, s):
    norm_to_hT(k, l, s, None)


def ffn(k, l, s):
    S = k.S
    C = Carver(k.R, k.RN)
    sub = 0 if s == 0 else 2
    gb = l * NMODC + sub * 3 * NJ + 2 * NJ
    gsrc = k.modT[:, gb:gb + NJ]
    S.op('dve', lambda h: h.tensor_scalar(out=k.colG[:], in0=gsrc, scalar1=0.5, scalar2=None, op0=ALU.mult),
         reads=['modT'], writes=['colG'])
    GS = 4
    NG = NFC // GS
    NGU = 2
    act = [C.bf16(GS * TC).rearrange("p (c t) -> p c t", c=GS) for _ in range(2)]
    wgu = [(C.bf16(NJ * 256).rearrange("p (j f) -> p j f", j=NJ),
            C.bf16(NJ * 256).rearrange("p (j f) -> p j f", j=NJ)) for _ in range(NGU)]
    wdr = [C.bf16(D) for _ in range(2 * GS)]
    sil = [C.f32(512) for _ in range(2)]
    Wg = k.ffnw('g', l, s)
    Wu = k.ffnw('u', l, s)
    Wd = k.ffnw('d', l, s)
    st = {'pair': 0, 'evac': 0}

    def gateup(g):
        ab = act[g % 2]
        for pr in range(GS // 2):
            slot = st['pair'] % NGU
            st['pair'] += 1
            wgs, wus = wgu[slot]
            c0 = (g * GS + pr * 2) * 128
            sg = Wg[:, c0:c0 + 256].rearrange("(j p) f -> p j f", p=128)
            su = Wu[:, c0:c0 + 256].rearrange("(j p) f -> p j f", p=128)
            S.dma('pool', lambda h, o=wgs, i=sg: h.dma_start(out=o, in_=i), writes=[('wg', slot)])
            S.dma('pool', lambda h, o=wus, i=su: h.dma_start(out=o, in_=i), writes=[('wu', slot)])
            if pr == 0:
                for ci in range(GS):
                    ws = (g % 2) * GS + ci
                    r0 = (g * GS + ci) * 128
                    S.dma('pool', lambda h, o=wdr[ws], r0=r0: h.dma_start(out=o, in_=Wd[r0:r0 + 128, :]),
                          writes=[('wd', ws)])
            for cc in range(2):
                ci = pr * 2 + cc
                for hf in range(2):
                    ts = slice(hf * 512, (hf + 1) * 512)
                    pg = ps_bank(k, hf * 2)
                    pu = ps_bank(k, hf * 2 + 1)
                    for j in range(NJ):
                        S.op('pe', lambda h, pg=pg, wgs=wgs, j=j, cc=cc, ts=ts: h.matmul(
                            pg, wgs[:, j, cc * 128:(cc + 1) * 128], k.hT[:, j, ts],
                            start=(j == 0), stop=(j == NJ - 1)),
                            reads=[('wg', slot), ('h', j, hf)], writes=[('psb', hf * 2)])
                    for j in range(NJ):
                        S.op('pe', lambda h, pu=pu, wus=wus, j=j, cc=cc, ts=ts: h.matmul(
                            pu, wus[:, j, cc * 128:(cc + 1) * 128], k.hT[:, j, ts],
                            start=(j == 0), stop=(j == NJ - 1)),
                            reads=[('wu', slot), ('h', j, hf)], writes=[('psb', hf * 2 + 1)])
                    sl = sil[hf]
                    S.op('act', lambda h, sl=sl, pg=pg: h.activation(out=sl, in_=pg, func=AF.Silu),
                         reads=[('psb', hf * 2)], writes=[('sil', hf)])
                    S.op('dve', lambda h, sl=sl, pu=pu, ci=ci, ts=ts, ab=ab: h.tensor_tensor(
                        out=ab[:, ci, ts], in0=pu, in1=sl, op=ALU.mult),
                        reads=[('psb', hf * 2 + 1), ('sil', hf)], writes=[('act', g % 2, ci, hf)])

    def down(g):
        ab = act[g % 2]
        for j in range(NJ):
            for hf in range(2):
                ts = slice(hf * 512, (hf + 1) * 512)
                b = 4 + st['evac'] % 4
                st['evac'] += 1
                pd = ps_bank(k, b)
                for ci in range(GS):
                    ws = (g % 2) * GS + ci
                    S.op('pe', lambda h, pd=pd, ws=ws, j=j, ci=ci, ts=ts, ab=ab: h.matmul(
                        pd, wdr[ws][:, j * 128:(j + 1) * 128], ab[:, ci, ts],
                        start=(ci == 0), stop=(ci == GS - 1)),
                        reads=[('wd', ws), ('act', g % 2, ci, hf)], writes=[('psb', b)])
                S.op('dve', lambda h, pd=pd, j=j, ts=ts: h.scalar_tensor_tensor(
                    out=k.xT[:, j, ts], in0=pd, scalar=k.colG[:, j:j + 1], in1=k.xT[:, j, ts],
                    op0=ALU.mult, op1=ALU.add),
                    reads=[('psb', b), 'colG', ('x', j, hf)], writes=[('x', j, hf)])

    for g in range(NG):
        gateup(g)
        if g > 0:
            down(g - 1)
    down(NG - 1)


def norm_stats(k, C):
    S = k.S
    sq = [C.bf16(512) for _ in range(2)]
    rstd = [C.f32(512) for _ in range(2)]
    for hf in range(2):
        ts = slice(hf * 512, (hf + 1) * 512)
        psq = ps_bank(k, 4 + hf)
        for j in range(NJ):
            q = sq[j % 2]
            S.op('act', lambda h, q=q, j=j, ts=ts: h.activation(out=q, in_=k.xT[:, j, ts], func=AF.Square),
                 reads=[('x', j, hf)], writes=[('sq', j % 2)])
            S.op('pe', lambda h, q=q, j=j, psq=psq: h.matmul(psq, k.ones16[:], q, start=(j == 0), stop=(j == NJ - 1)),
                 reads=[('sq', j % 2), 'ones16'], writes=[('psb', 4 + hf)])
        r = rstd[hf]
        S.op('act', lambda h, r=r, psq=psq: h.activation(out=r, in_=psq, func=AF.Sqrt, bias=k.epsc[:, 0:1], scale=1.0),
             reads=[('psb', 4 + hf), 'epsc'], writes=[('rstd', hf)])
        S.op('dve', lambda h, r=r: h.reciprocal(out=r, in_=r), reads=[('rstd', hf)], writes=[('rstd', hf)])
    return rstd


def mod_cols(k, l, s):
    S = k.S
    base = l * NMODC + s * 3 * NJ
    sh = k.modT[:, base:base + NJ]
    sc = k.modT[:, base + NJ:base + 2 * NJ]
    gate = k.modT[:, base + 2 * NJ:base + 3 * NJ]
    gcol = k.gT[:, (l * 3 + s) * NJ:(l * 3 + s + 1) * NJ]
    S.op('dve', lambda h: h.tensor_scalar(out=k.colA[:], in0=sc, scalar1=1.0, scalar2=float(np.sqrt(D)),
                                          op0=ALU.add, op1=ALU.mult), reads=['modT'], writes=['colA'])
    S.op('dve', lambda h: h.tensor_tensor(out=k.colA[:], in0=k.colA[:], in1=gcol, op=ALU.mult),
         reads=['colA', 'gT0', 'gT1'], writes=['colA'])
    return sh, gate


def pool_A(k, l):
    S = k.S
    S.barrier()
    C = Carver(k.R, k.RN)
    sh, gate = mod_cols(k, l, 1)
    rstd = norm_stats(k, C)
    HB = 16
    halo_src = C.f32(NJ * HB).rearrange("p (j t) -> p j t", j=NJ)
    t16 = C.f32(HB)
    for j in range(NJ):
        S.op('dve', lambda h, j=j: h.tensor_tensor(out=t16, in0=k.xT[:, j, TC - HB:TC], in1=rstd[1][:, 512 - HB:512],
                                                  op=ALU.mult),
             reads=[('x', j, 1), ('rstd', 1)], writes=['t16'])
        S.op('act', lambda h, j=j: h.activation(out=halo_src[:, j, :], in_=t16, func=AF.Identity,
                                                bias=sh[:, j:j + 1], scale=k.colA[:, j:j + 1]),
             reads=['t16', 'colA', 'modT'], writes=['halo_src'])
    hl = k.ex_w('phalo', NJ * HB)
    S.dma('pool', lambda h: h.dma_start(out=hl, in_=halo_src.rearrange("p j t -> p (j t)")),
          reads=['halo_src'], writes=['exo'])
    k.exchange('phalo', NJ * HB, ['exo'], 'halo_all_d')
    S.barrier()


def pool_B(k, l):
    S = k.S
    S.barrier()
    C = Carver(k.R, k.RN)
    sh, gate = mod_cols(k, l, 1)
    S.op('dve', lambda h: h.tensor_tensor(out=k.colG[:], in0=gate, in1=k.psT[:], op=ALU.mult),
         reads=['modT', 'psT'], writes=['colG'])
    rstd = norm_stats(k, C)
    HB = 16
    halo_all = C.f32(NCORES * NJ * HB).rearrange("p (r c) -> p r c", r=NCORES)
    halo = C.f32(NJ * HB).rearrange("p (j t) -> p j t", j=NJ)
    hallg = k.ex_all('phalo', NJ * HB)
    S.dma('pool', lambda h: h.dma_start(out=halo_all, in_=hallg.rearrange("(r p) c -> p r c", p=128)),
          reads=['halo_all_d'], writes=['halo_all'])
    hflat = halo.rearrange("p j t -> p (j t)")
    S.op('dve', lambda h: h.tensor_scalar(out=hflat, in0=halo_all[:, 0, :], scalar1=k.onehot[:, 0:1], scalar2=None,
                                          op0=ALU.mult), reads=['halo_all', 'onehot'], writes=['halo'])
    for r in range(1, NCORES):
        S.op('dve', lambda h, r=r: h.scalar_tensor_tensor(out=hflat, in0=halo_all[:, r, :], scalar=k.onehot[:, r:r + 1],
                                                         in1=hflat, op0=ALU.mult, op1=ALU.add),
             reads=['halo_all', 'onehot', 'halo'], writes=['halo'])
    wp = [C.bf16(4 * 512).rearrange("p (i o) -> p i o", i=4) for _ in range(4)]
    for g in range(4):
        S.dma('pool', lambda h, g=g: h.dma_start(out=wp[g], in_=k.pool_w[g].rearrange("(i p) o -> p i o", p=128)),
              writes=[('wp', g)])
    W = HB + 512
    hb = [C.f32(W) for _ in range(2)]
    pp = [C.f32(W) for _ in range(2)]
    qq = [C.f32(W) for _ in range(2)]
    tmp = [C.f32(512) for _ in range(2)]
    fix = C.f32(HB)
    it = 0
    for j in range(NJ):
        L = 1 + j // 4
        w = 2 ** L
        for hf in (1, 0):
            ts = slice(hf * 512, (hf + 1) * 512)
            b = hb[it % 2]
            p_ = pp[it % 2]
            q_ = qq[it % 2]
            t = tmp[it % 2]
            sfx = it % 2
            it += 1
            S.op('dve', lambda h, t=t, j=j, ts=ts, hf=hf: h.tensor_tensor(out=t, in0=k.xT[:, j, ts], in1=rstd[hf], op=ALU.mult),
                 reads=[('x', j, hf), ('rstd', hf)], writes=[('ptmp', sfx)])
            S.op('act', lambda h, t=t, j=j, b=b: h.activation(out=b[:, HB:W], in_=t, func=AF.Identity,
                                                             bias=sh[:, j:j + 1], scale=k.colA[:, j:j + 1]),
                 reads=[('ptmp', sfx), 'colA', 'modT'], writes=[('hb', sfx)])
            if hf == 1:
                S.op('dve', lambda h, t=t, j=j: h.tensor_tensor(out=t[:, 0:HB], in0=k.xT[:, j, 512 - HB:512],
                                                               in1=rstd[0][:, 512 - HB:512], op=ALU.mult),
                     reads=[('x', j, 0), ('rstd', 0), ('ptmp', sfx)], writes=[('ptmp', sfx)])
                S.op('act', lambda h, t=t, j=j, b=b: h.activation(out=b[:, 0:HB], in_=t[:, 0:HB], func=AF.Identity,
                                                                 bias=sh[:, j:j + 1], scale=k.colA[:, j:j + 1]),
                     reads=[('ptmp', sfx), 'colA', 'modT'], writes=[('hb', sfx)])
            else:
                S.op('act', lambda h, j=j, b=b: h.copy(out=b[:, 0:HB], in_=halo[:, j, :]),
                     reads=['halo'], writes=[('hb', sfx)])
            src = b
            bufs = [p_, q_]
            for lev in range(L):
                sft = 2 ** lev
                dst = bufs[lev % 2]
                S.op('dve', lambda h, src=src, dst=dst, sft=sft: h.tensor_tensor(
                    out=dst[:, sft:W], in0=src[:, sft:W], in1=src[:, 0:W - sft], op=ALU.add),
                    reads=[('hb', sfx), ('pq', sfx)], writes=[('pq', sfx)])
                src = dst
            S.op('dve', lambda h, src=src, b=b, j=j, ts=ts, w=w: h.scalar_tensor_tensor(
                out=k.hT[:, j, ts], in0=src[:, HB:W], scalar=1.0 / w, in1=b[:, HB:W],
                op0=ALU.mult, op1=ALU.subtract),
                reads=[('pq', sfx), ('hb', sfx)], writes=[('h', j, hf)])
            if hf == 0:
                S.op('dve', lambda h, src=src, L=L: h.tensor_tensor(out=fix, in0=src[:, HB:2 * HB],
                                                                   in1=k.pinvc[:, (L - 1) * HB:L * HB], op=ALU.mult),
                     reads=[('pq', sfx), 'pinvc'], writes=['fix'])
                S.op('dve', lambda h, b=b, j=j: h.tensor_tensor(out=k.hT[:, j, 0:HB], in0=fix, in1=b[:, HB:2 * HB],
                                                               op=ALU.subtract),
                     reads=['fix', ('hb', sfx), ('h', j, 0)], writes=[('h', j, 0)])
    ev = 0
    for g in range(4):
        for oc in range(4):
            j = g * 4 + oc
            for hf in range(2):
                ts = slice(hf * 512, (hf + 1) * 512)
                bnk = ev % 4
                ev += 1
                pd = ps_bank(k, bnk)
                for ic in range(4):
                    S.op('pe', lambda h, pd=pd, g=g, ic=ic, oc=oc, ts=ts: h.matmul(
                        pd, wp[g][:, ic, oc * 128:(oc + 1) * 128], k.hT[:, g * 4 + ic, ts],
                        start=(ic == 0), stop=(ic == 3)),
                        reads=[('wp', g), ('h', g * 4 + ic, hf)], writes=[('psb', bnk)])
                S.op('dve', lambda h, pd=pd, j=j, ts=ts: h.scalar_tensor_tensor(
                    out=k.xT[:, j, ts], in0=pd, scalar=k.colG[:, j:j + 1], in1=k.xT[:, j, ts],
                    op0=ALU.mult, op1=ALU.add),
                    reads=[('psb', bnk), 'colG', ('x', j, hf)], writes=[('x', j, hf)])
    S.barrier()


def norm_to_hT(k, l, s, C):
    S = k.S
    sh, gate = mod_cols(k, l, s)
    C = Carver(k.R, k.RN)
    C.reset(k.RN - 3072)
    rstd = norm_stats(k, C)
    tmp = [C.f32(512) for _ in range(3)]
    for hf in range(2):
        ts = slice(hf * 512, (hf + 1) * 512)
        for j in range(NJ):
            t = tmp[j % 3]
            S.op('dve', lambda h, t=t, j=j, ts=ts, hf=hf: h.tensor_tensor(out=t, in0=k.xT[:, j, ts], in1=rstd[hf], op=ALU.mult),
                 reads=[('x', j, hf), ('rstd', hf)], writes=[('ntmp', j % 3)])
            S.op('act', lambda h, t=t, j=j, ts=ts: h.activation(out=k.hT[:, j, ts], in_=t, func=AF.Identity,
                                                          bias=sh[:, j:j + 1], scale=k.colA[:, j:j + 1]),
                 reads=[('ntmp', j % 3), 'colA', 'modT'], writes=[('h', j, hf)])
    return gate


def swa_layout(k):
    C = Carver(k.R, k.RN)
    NT = TC // 128
    L = {}
    L['QT'] = C.bf16(NJ * TC).rearrange("p (c t) -> p c t", c=NJ)
    L['KT'] = C.bf16(4 * (128 + TC)).rearrange("p (v t) -> p v t", v=4)
    L['VA'] = C.bf16((NT + 1) * 4 * 65).rearrange("p (n v e) -> p n v e", n=NT + 1, v=4)
    L['tab'] = C.f32(96)
    L['esink'] = C.f32(32)
    L['slq'] = C.bf16(32 * 128).rearrange("p (a q) -> p a q", a=32)
    L['cm'] = C.bf16(3 * 128).rearrange("p (a q) -> p a q", a=3)
    HW = 256 + 130
    L['hsrc'] = C.f32(HW)
    return C, L


def swa_A(k, l):
    S = k.S
    S.barrier()
    Wqkv = k.swa_qkv
    NT = TC // 128
    gate = norm_to_hT(k, l, 1, None)
    S.op('dve', lambda h: h.tensor_copy(out=k.colG[:], in_=gate), reads=['modT'], writes=['colG'])
    C, L = swa_layout(k)
    QT, KT, VA, tab, esink, slq, cm, hsrc = (L[x] for x in ('QT', 'KT', 'VA', 'tab', 'esink', 'slq', 'cm', 'hsrc'))
    S.dma('sp', lambda h: h.dma_start(out=tab, in_=k.swa_tab), writes=['swtab'])
    S.dma('sp', lambda h: h.dma_start(out=slq[0:3, :, :], in_=k.swa_slq), writes=['slq'])
    S.dma('sp', lambda h: h.dma_start(out=cm, in_=k.swa_cm), writes=['cm'])
    S.op('act', lambda h: h.activation(out=esink, in_=tab[:, 64:96], func=AF.Exp), reads=['swtab'], writes=['esink'])
    S.op('dve', lambda h: h.memset(VA[:, :, :, 64:65], 1.0), writes=['VAones'])
    pmark = C.mark()
    wring = [C.bf16(NJ * 128).rearrange("p (j f) -> p j f", j=NJ) for _ in range(4)]
    wst = {'n': 0}

    def wslot():
        i = wst['n'] % 4
        wst['n'] += 1
        return wring[i], i
    ev = 0
    for v in range(4):
        wk, ws = wslot()
        src = Wqkv[:, D + v * 64:D + (v + 1) * 64].rearrange("(j p) f -> p j f", p=128)
        for e in range(2):
            S.dma('pool', lambda h, wk=wk, e=e, src=src: h.dma_start(out=wk[:, :, e * 64:(e + 1) * 64], in_=src),
                  writes=[('wring', ws, e)])
        for hf in range(2):
            b = ev % 4
            ev += 1
            pk = ps_bank(k, b)
            for j in range(NJ):
                S.op('pe', lambda h, pk=pk, wk=wk, j=j, hf=hf: h.matmul(
                    pk, wk[:, j, :], k.hT[:, j, hf * 512:(hf + 1) * 512], start=(j == 0), stop=(j == NJ - 1)),
                    reads=[('wring', ws, 0), ('wring', ws, 1), ('h', j, hf)], writes=[('psb', b)])
            S.op('act', lambda h, pk=pk, v=v, hf=hf: h.copy(out=KT[:, v, 128 + hf * 512:128 + (hf + 1) * 512], in_=pk),
                 reads=[('psb', b)], writes=[('KT', v, hf)])
    wv, wvs = wslot()
    for e in range(2):
        S.dma('pool', lambda h, e=e: h.dma_start(
            out=wv[:, :, e * 64:(e + 1) * 64],
            in_=Wqkv[:, D + 256:D + 512].rearrange("(j p) f -> p j f", p=128)[:, :, e * 64:(e + 1) * 64]),
            writes=[('wring', wvs, e)])
    wv2, wvs2 = wslot()
    for e in range(2):
        S.dma('pool', lambda h, e=e: h.dma_start(
            out=wv2[:, :, e * 64:(e + 1) * 64],
            in_=Wqkv[:, D + 256:D + 512].rearrange("(j p) f -> p j f", p=128)[:, :, 128 + e * 64:128 + (e + 1) * 64]),
            writes=[('wring', wvs2, e)])
    for tt in range(NT):
        b = ev % 4
        ev += 1
        pv = ps_bank(k, b, 256)
        for vh, (wvx, wsx) in enumerate([(wv, wvs), (wv2, wvs2)]):
            for j in range(NJ):
                S.op('pe', lambda h, pv=pv, tt=tt, j=j, wvx=wvx, vh=vh: h.matmul(
                    pv[:, vh * 128:(vh + 1) * 128], k.hT[:, j, tt * 128:(tt + 1) * 128], wvx[:, j, :],
                    start=(j == 0), stop=(j == NJ - 1)),
                    reads=[('wring', wsx, 0), ('wring', wsx, 1), ('h', j, tt // 4)], writes=[('psb', b)])
        S.op('dve', lambda h, pv=pv, tt=tt: h.tensor_copy(out=VA[:, tt + 1, :, 0:64],
                                                          in_=pv.rearrange("p (v e) -> p v e", v=4)),
             reads=[('psb', b), 'VAones'], writes=[('VA', tt + 1)])
    for qc in range(NJ):
        w, ws = wslot()
        S.dma('pool', lambda h, w=w, qc=qc: h.dma_start(
            out=w, in_=Wqkv[:, qc * 128:(qc + 1) * 128].rearrange("(j p) f -> p j f", p=128)),
            writes=[('wring', ws, 0), ('wring', ws, 1)])
        if True:
            for hf in range(2):
                b = ev % 4
                ev += 1
                pq = ps_bank(k, b)
                for j in range(NJ):
                    S.op('pe', lambda h, pq=pq, w=w, j=j, hf=hf: h.matmul(
                        pq, w[:, j, :], k.hT[:, j, hf * 512:(hf + 1) * 512],
                        start=(j == 0), stop=(j == NJ - 1)),
                        reads=[('wring', ws, 0), ('wring', ws, 1), ('h', j, hf)], writes=[('psb', b)])
                if (qc + hf) % 2 == 0:
                    S.op('act', lambda h, pq=pq, qc=qc, hf=hf: h.copy(out=QT[:, qc, hf * 512:(hf + 1) * 512], in_=pq),
                         reads=[('psb', b)], writes=[('QT', qc, hf)])
                else:
                    S.op('dve', lambda h, pq=pq, qc=qc, hf=hf: h.tensor_copy(out=QT[:, qc, hf * 512:(hf + 1) * 512], in_=pq),
                         reads=[('psb', b)], writes=[('QT', qc, hf)])
    HW = 256 + 130
    hsrc16 = hsrc.bitcast(BF16)
    S.op('dve', lambda h: h.tensor_copy(out=hsrc16[:, 0:512].rearrange("p (v t) -> p v t", v=4), in_=KT[:, :, TC:TC + 128]),
         reads=[('KT', v, 1) for v in range(4)], writes=['hsrcK'])
    S.op('dve', lambda h: h.tensor_copy(out=hsrc16[:, 512:772], in_=VA[:, NT, :, :].rearrange("p v e -> p (v e)")),
         reads=[('VA', NT), 'VAones'], writes=['hsrcV'])
    shl = k.ex_w('shalo', HW)
    S.dma('pool', lambda h: h.dma_start(out=shl, in_=hsrc), reads=['hsrcK', 'hsrcV'], writes=['exo'])
    k.exchange('shalo', HW, ['exo'], 'swa_ha_d')
    S.barrier()


def swa_B(k, l):
    S = k.S
    S.barrier()
    Wo = k.swa_o
    NT = TC // 128
    SC = 0.125
    HW = 256 + 130
    C, L = swa_layout(k)
    QT, KT, VA, tab, esink, slq, cm, hsrc = (L[x] for x in ('QT', 'KT', 'VA', 'tab', 'esink', 'slq', 'cm', 'hsrc'))
    hall = C.f32(NCORES * HW).rearrange("p (r c) -> p r c", r=NCORES)
    hsel = C.f32(HW)
    shall = k.ex_all('shalo', HW)
    S.dma('pool', lambda h: h.dma_start(out=hall, in_=shall.rearrange("(r p) c -> p r c", p=128)),
          reads=['swa_ha_d'], writes=['hall'])
    hall16 = [hall[:, r, :].bitcast(BF16) for r in range(NCORES)]
    hsel16 = hsel.bitcast(BF16)
    S.op('dve', lambda h: h.tensor_scalar(out=hsel16, in0=hall16[0], scalar1=k.onehot[:, 0:1], scalar2=None, op0=ALU.mult),
         reads=['hall', 'onehot'], writes=['hsel'])
    for r in range(1, NCORES):
        S.op('dve', lambda h, r=r: h.scalar_tensor_tensor(out=hsel16, in0=hall16[r], scalar=k.onehot[:, r:r + 1], in1=hsel16,
                                                         op0=ALU.mult, op1=ALU.add),
             reads=['hall', 'onehot', 'hsel'], writes=['hsel'])
    S.op('dve', lambda h: h.tensor_copy(out=KT[:, :, 0:128], in_=hsel16[:, 0:512].rearrange("p (v t) -> p v t", v=4)),
         reads=['hsel'], writes=['KThalo'])
    S.op('dve', lambda h: h.tensor_copy(out=VA[:, 0, :, :].rearrange("p v e -> p (v e)"), in_=hsel16[:, 512:772]),
         reads=['hsel', 'VAones'], writes=[('VA', 0)])
    PT = [C.bf16(256).rearrange("p (a q) -> p a q", a=2) for _ in range(3)]
    Ost = [C.bf16(D) for _ in range(2)]
    den = [C.f32(1) for _ in range(4)]
    PSB16 = [k.PS[:, 6 * 512:7 * 512].bitcast(BF16), k.PS[:, 7 * 512:8 * 512].bitcast(BF16)]
    it = 0
    for qt in range(NT):
        os_ = Ost[qt % 2]
        for hq in range(32):
            v = hq // 8
            e = hq % 2
            qc = hq // 2
            pr = slice(e * 64, (e + 1) * 64)
            b = (it % 2) * 2
            ob = 4 + (it % 2)
            pt = PT[it % 3]
            dn = den[it % 4]
            sfx = it
            it += 1
            ps = [ps_bank(k, b, 128), ps_bank(k, b + 1, 128)]
            po = ps_bank(k, ob, 65)
            for a in range(2):
                kcols = slice(qt * 128 + a * 128, qt * 128 + a * 128 + 128)
                mi = (2 if qt == 0 else 0) if a == 0 else 1
                rd = [('KT', v, 0), ('KT', v, 1), 'KThalo', ('QT', qc, qt // 4)]
                S.op('pe', lambda h, ps=ps, a=a, v=v, pr=pr, kcols=kcols, qc=qc, qt=qt: h.matmul(
                    ps[a], KT[pr, v, kcols], QT[pr, qc, qt * 128:(qt + 1) * 128], start=True, stop=False),
                    reads=rd, writes=[('psb', b + a)])
                S.op('pe', lambda h, ps=ps, a=a, mi=mi: h.matmul(
                    ps[a], k.ident16[:], cm[:, mi, :], start=False, stop=False),
                    reads=['ident16', 'cm'], writes=[('psb', b + a)])
                S.op('pe', lambda h, ps=ps, a=a, hq=hq: h.matmul(
                    ps[a], k.ones16[0:3, :], slq[0:3, hq, :], start=False, stop=True),
                    reads=['ones16', 'slq'], writes=[('psb', b + a)])
                S.op('act', lambda h, ps=ps, a=a, pt=pt, hq=hq: h.activation(
                    out=pt[:, a, :], in_=ps[a], func=AF.Exp, bias=tab[:, a * 32 + hq:a * 32 + hq + 1], scale=SC),
                    reads=[('psb', b + a), 'swtab'], writes=[('PT', it % 3, a)])
            for a in range(2):
                S.op('pe', lambda h, po=po, pt=pt, a=a, qt=qt, v=v: h.matmul(
                    po, pt[:, a, :], VA[:, qt + a, v, :], start=(a == 0), stop=(a == 1)),
                    reads=[('PT', it % 3, a), ('VA', qt + a), 'VAones'], writes=[('psb', ob)])
            S.op('dve', lambda h, dn=dn, po=po, hq=hq: h.tensor_tensor(out=dn, in0=po[:, 64:65], in1=esink[:, hq:hq + 1], op=ALU.add),
                 reads=[('psb', ob), 'esink'], writes=[('den', it % 4)])
            S.op('dve', lambda h, dn=dn: h.reciprocal(out=dn, in_=dn), reads=[('den', it % 4)], writes=[('den', it % 4)])
            S.op('dve', lambda h, dn=dn, po=po, hq=hq, os_=os_: h.tensor_scalar(
                out=os_[:, hq * 64:(hq + 1) * 64], in0=po[:, 0:64], scalar1=dn[:, 0:1], scalar2=None, op0=ALU.mult),
                reads=[('psb', ob), ('den', it % 4)], writes=[('Ost', qt % 2, hq // 2)])
        for qc in range(NJ):
            S.op('pe', lambda h, qc=qc, os_=os_: h.transpose(out=PSB16[qc % 2][:, 0:128],
                                                            in_=os_[:, qc * 128:(qc + 1) * 128], identity=k.ident16[:]),
                 reads=[('Ost', qt % 2, qc), 'ident16'], writes=[('psb', 6 + qc % 2)])
            S.op('act', lambda h, qc=qc, qt=qt: h.copy(out=k.hT[:, qc, qt * 128:(qt + 1) * 128],
                                                       in_=PSB16[qc % 2][:, 0:128]),
                 reads=[('psb', 6 + qc % 2)], writes=[('h', qc, qt // 4)])
    S.barrier()
    C2 = Carver(k.R, k.RN)
    proj_out(k, Wo, C2)
    S.barrier()


def proj_out(k, Wo, C):
    S = k.S
    NS = 4
    wo = [C.bf16(D) for _ in range(NS)]
    for jq in range(4):
        for r in range(NJ):
            slot = (jq * NJ + r) % NS
            S.dma('pool', lambda h, slot=slot, r=r, jq=jq: h.dma_start(
                out=wo[slot][:, 0:512], in_=Wo[r * 128:(r + 1) * 128, jq * 512:(jq + 1) * 512]),
                writes=[('wo', slot)])
            for jj in range(4):
                for hf in range(2):
                    b = jj * 2 + hf
                    S.op('pe', lambda h, b=b, slot=slot, jj=jj, r=r, hf=hf: h.matmul(
                        ps_bank(k, b), wo[slot][:, jj * 128:(jj + 1) * 128], k.hT[:, r, hf * 512:(hf + 1) * 512],
                        start=(r == 0), stop=(r == NJ - 1)),
                        reads=[('wo', slot), ('h', r, hf)], writes=[('psb', b)])
        for jj in range(4):
            j = jq * 4 + jj
            for hf in range(2):
                b = jj * 2 + hf
                ts = slice(hf * 512, (hf + 1) * 512)
                S.op('dve', lambda h, b=b, j=j, ts=ts: h.scalar_tensor_tensor(
                    out=k.xT[:, j, ts], in0=ps_bank(k, b), scalar=k.colG[:, j:j + 1], in1=k.xT[:, j, ts],
                    op0=ALU.mult, op1=ALU.add),
                    reads=[('psb', b), 'colG', ('x', j, hf)], writes=[('x', j, hf)])


KVW = 8192 + 8256 + 64
MSC = float(128 ** -0.5)


def moba_A(k, l, jm):
    S = k.S
    S.barrier()
    C = Carver(k.R, k.RN)
    Wqkv = k.mobaw('qkv', jm)
    NT = TC // 128
    gate = norm_to_hT(k, l, 1, C)
    S.op('dve', lambda h: h.tensor_copy(out=k.colG[:], in_=gate), reads=['modT'], writes=['colG'])
    QT = C.bf16(16 * TC).rearrange("p (c t) -> p c t", c=16)
    base = C.mark()
    wr = [C.bf16(NJ * 256).rearrange("p (j f) -> p j f", j=NJ) for _ in range(3)]
    kst32 = [C.f32(512) for _ in range(2)]
    vst32 = [C.f32(1032) for _ in range(2)]
    kms = C.f32(64)
    st = {'w': 0, 'ev': 0}
    kv_keys = []

    def load_w(c0):
        slot = st['w'] % 3
        st['w'] += 1
        w = wr[slot]
        S.dma('pool', lambda h, w=w, c0=c0: h.dma_start(
            out=w, in_=Wqkv[:, c0:c0 + 256].rearrange("(j p) f -> p j f", p=128)), writes=[('wr', slot)])
        return w, slot

    pending = []

    def flush_ex():
        while pending:
            h2 = pending.pop(0)
            k.exchange('kv%d' % h2, 1028, [('kvl', 'K', h2), ('kvl', 'V', h2)], ('kvall', h2))
    for vb in range(2):
        v16 = vst32[vb].bitcast(BF16).rearrange("p (h n e) -> p h n e", h=2, n=NT)
        S.op('dve', lambda h, v16=v16: h.memset(v16[:, :, :, 128:129], 1.0), writes=[('vones', vb)])
    for hp in range(8):
        w, slot = load_w(hp * 256)
        for cc in range(2):
            hh = hp * 2 + cc
            for hf in range(2):
                b = st['ev'] % 4
                st['ev'] += 1
                pq = ps_bank(k, b)
                for j in range(NJ):
                    S.op('pe', lambda h, pq=pq, w=w, cc=cc, j=j, hf=hf: h.matmul(
                        pq, w[:, j, cc * 128:(cc + 1) * 128], k.hT[:, j, hf * 512:(hf + 1) * 512],
                        start=(j == 0), stop=(j == NJ - 1)),
                        reads=[('wr', slot), ('h', j, hf)], writes=[('psb', b)])
                S.op('act', lambda h, pq=pq, hh=hh, hf=hf: h.copy(out=QT[:, hh, hf * 512:(hf + 1) * 512], in_=pq),
                     reads=[('psb', b)], writes=[('QT', hh, hf)])
        w, slot = load_w(D + hp * 256)
        for cc in range(2):
            hh = hp * 2 + cc
            kb = hh % 2
            k16 = kst32[kb].bitcast(BF16)
            for hf in range(2):
                b = st['ev'] % 4
                st['ev'] += 1
                pk = ps_bank(k, b)
                for j in range(NJ):
                    S.op('pe', lambda h, pk=pk, w=w, cc=cc, j=j, hf=hf: h.matmul(
                        pk, w[:, j, cc * 128:(cc + 1) * 128], k.hT[:, j, hf * 512:(hf + 1) * 512],
                        start=(j == 0), stop=(j == NJ - 1)),
                        reads=[('wr', slot), ('h', j, hf)], writes=[('psb', b)])
                S.op('act', lambda h, pk=pk, k16=k16, hf=hf: h.copy(out=k16[:, hf * 512:(hf + 1) * 512], in_=pk),
                     reads=[('psb', b)], writes=[('kst', kb, hf)])
                S.op('dve', lambda h, k16=k16, hh=hh, hf=hf: h.tensor_reduce(
                    out=kms[:, hh * 4 + hf * 2:hh * 4 + hf * 2 + 2],
                    in_=k16[:, hf * 512:(hf + 1) * 512].rearrange("p (b t) -> p b t", b=2),
                    axis=AX.X, op=ALU.add), reads=[('kst', kb, hf)], writes=['kms'])
            S.dma('sp', lambda h, kb=kb, hh=hh: h.dma_start(out=k.kv_w(hh)[0], in_=kst32[kb]),
                  reads=[('kst', kb, 0), ('kst', kb, 1)], writes=[('kvl', 'K', hh)])
            kv_keys.append(('kvl', 'K', hh))
        w, slot = load_w(2 * D + hp * 256)
        flush_ex()
        vb = hp % 2
        v16 = vst32[vb].bitcast(BF16).rearrange("p (h n e) -> p h n e", h=2, n=NT)
        for tt in range(NT):
            b = st['ev'] % 4
            st['ev'] += 1
            pv = ps_bank(k, b, 256)
            for j in range(NJ):
                S.op('pe', lambda h, pv=pv, w=w, j=j, tt=tt: h.matmul(
                    pv, k.hT[:, j, tt * 128:(tt + 1) * 128], w[:, j, :], start=(j == 0), stop=(j == NJ - 1)),
                    reads=[('wr', slot), ('h', j, tt // 4)], writes=[('psb', b)])
            S.op('dve', lambda h, pv=pv, v16=v16, tt=tt: h.tensor_copy(
                out=v16[:, :, tt, 0:128], in_=pv.rearrange("p (h e) -> p h e", h=2)),
                reads=[('psb', b), ('vones', vb)], writes=[('vst', vb)])
        for e2 in range(2):
            hh2 = hp * 2 + e2
            S.dma('sp', lambda h, vb=vb, hh2=hh2, e2=e2: h.dma_start(
                out=k.kv_w(hh2)[1], in_=vst32[vb][:, e2 * 516:(e2 + 1) * 516]),
                reads=[('vst', vb), ('vones', vb)], writes=[('kvl', 'V', hh2)])
            kv_keys.append(('kvl', 'V', hh2))
            pending.append(hh2)
    S.op('dve', lambda h: h.tensor_scalar(out=kms, in0=kms, scalar1=1.0 / 256.0, scalar2=None, op0=ALU.mult),
         reads=['kms'], writes=['kms'])
    flush_ex()
    S.dma('sp', lambda h: h.dma_start(out=k.km_w(), in_=kms), reads=['kms'], writes=[('kvl', 'M')])
    kv_keys.append(('kvl', 'M'))
    k.exchange('km', 64, [('kvl', 'M')], 'km_all')
    S.op('sp', lambda h: h.nop(), reads=kv_keys, writes=['exo'])
    S.barrier()


def moba_B(k, l, jm):
    S = k.S
    S.barrier()
    Wo = k.mobaw('o', jm)
    NT = TC // 128
    C = Carver(k.R, k.RN)
    QT = C.bf16(16 * TC).rearrange("p (c t) -> p c t", c=16)
    km32 = C.f32(NCORES * 64).rearrange("p (r c) -> p r c", r=NCORES)
    km16 = C.bf16(NCORES * 64).rearrange("p (r c) -> p r c", r=NCORES)
    pastb = C.f32(128).rearrange("p (a n) -> p a n", a=4)
    TL = C.bf16(32 * 128)
    cmask = C.bf16(512).rearrange("p (a q) -> p a q", a=2)
    bown = C.f32(32)
    TR = [C.bf16(1024).rearrange("p (a q) -> p a q", a=2) for _ in range(2)]
    btab = [C.f32(128) for _ in range(2)]
    Kc = [C.f32(1024) for _ in range(3)]
    Vc = [C.f32(1032) for _ in range(3)]
    KL = [C.f32(512) for _ in range(2)]
    VL = [C.f32(516) for _ in range(2)]
    PT = [C.bf16(512) for _ in range(3)]
    gm = [C.f32(32) for _ in range(2)]
    top8 = [C.f32(8) for _ in range(2)]
    thr = [C.f32(1) for _ in range(2)]
    selb = [C.bf16(32) for _ in range(2)]
    On = [C.bf16(128) for _ in range(2)]
    rden = [C.f32(1) for _ in range(4)]
    S.dma('sp', lambda h: h.dma_start(out=km32, in_=k.km_all()), reads=['km_all'], writes=['km32'])
    S.op('dve', lambda h: h.tensor_copy(out=km16, in_=km32), reads=['km32'], writes=['km16'])
    S.dma('sp', lambda h: h.dma_start(out=pastb, in_=k.moba_pastb.rearrange("p (a n) -> p a n", a=4)), writes=['pastb'])
    S.op('dve', lambda h: h.memset(TL, 0.0), writes=['TL'])
    for tr_ in TR:
        S.op('dve', lambda h, tr_=tr_: h.memset(tr_, 0.0), writes=[('TRz', id(tr_))])
    S.dma('sp', lambda h: h.dma_start(out=TL[0:35, :], in_=k.moba_TL), reads=['TL'], writes=['TL'])
    S.dma('sp', lambda h: h.dma_start(out=cmask, in_=k.moba_cmask), writes=['cmask'])
    S.dma('sp', lambda h: h.dma_start(out=bown, in_=k.moba_bown), writes=['bown'])
    G7 = k.PS[:, 7 * 512:7 * 512 + 128]
    T7 = k.PS[:, 7 * 512 + 128:8 * 512].bitcast(BF16)
    acc = {}
    for qt in range(NT):
        bnk = 3 + qt // 2
        off = (qt % 2) * 256
        acc[qt] = (k.PS[:, bnk * 512 + off:bnk * 512 + off + 129], bnk)
    cnt = {'s': 0, 'p': 0, 'kv': 0, 'g': 0, 't': 0, 'o': 0}
    pend_pv = []
    for hh in range(16):
        sl = hh % 2
        tr = TR[sl]
        bt = btab[sl]
        kl = KL[sl]
        vl = VL[sl]
        kl16 = kl.bitcast(BF16)
        vl16 = vl.bitcast(BF16)
        S.dma('sp', lambda h, tr=tr, hh=hh: h.dma_start(out=tr[32:35, :, :], in_=k.moba_slq[hh]), writes=[('TRs', sl)])
        S.dma('sp', lambda h, bt=bt, hh=hh: h.dma_start(out=bt, in_=k.moba_bias[hh]), writes=[('bt', sl)])
        S.dma('sp', lambda h, kl=kl, hh=hh: h.dma_start(out=kl, in_=k.kv_own(hh)[0]),
              reads=[('kvl', 'K', hh)], writes=[('KL', sl)])
        S.dma('sp', lambda h, vl=vl, hh=hh: h.dma_start(out=vl, in_=k.kv_own(hh)[1]),
              reads=[('kvl', 'V', hh)], writes=[('VL', sl)])
        for qt in range(NT):
            gi = cnt['g'] % 4
            cnt['g'] += 1
            g2 = cnt['g'] % 2
            pg = G7[:, gi * 32:(gi + 1) * 32]
            S.op('pe', lambda h, pg=pg, hh=hh, qt=qt: h.matmul(
                pg, QT[:, hh, qt * 128:(qt + 1) * 128], km16[:, :, hh * 4:(hh + 1) * 4], start=True, stop=True),
                reads=[('QT', hh, qt // 4), 'km16'], writes=[('psb', 7)])
            S.op('dve', lambda h, pg=pg, g2=g2, qt=qt: h.tensor_tensor(out=gm[g2], in0=pg, in1=pastb[:, qt // 2, :], op=ALU.add),
                 reads=[('psb', 7), 'pastb'], writes=[('gm', g2)])
            S.op('dve', lambda h, g2=g2: h.max(out=top8[g2], in_=gm[g2]), reads=[('gm', g2)], writes=[('top8', g2)])
            S.op('dve', lambda h, g2=g2: h.tensor_scalar(out=thr[g2], in0=top8[g2][:, 2:3], scalar1=-1e29, scalar2=None, op0=ALU.max),
                 reads=[('top8', g2)], writes=[('thr', g2)])
            S.op('dve', lambda h, g2=g2: h.tensor_scalar(out=gm[g2], in0=gm[g2], scalar1=thr[g2][:, 0:1], scalar2=1.0,
                                                         op0=ALU.is_ge, op1=ALU.subtract),
                 reads=[('gm', g2), ('thr', g2)], writes=[('gm', g2)])
            S.op('dve', lambda h, g2=g2: h.tensor_scalar(out=selb[g2], in0=gm[g2], scalar1=BIG, scalar2=None, op0=ALU.mult),
                 reads=[('gm', g2)], writes=[('selb', g2)])
            ti = cnt['t'] % 6
            cnt['t'] += 1
            pt_ = T7[0:32, ti * 128:(ti + 1) * 128]
            S.op('pe', lambda h, pt_=pt_, g2=g2: h.transpose(out=pt_, in_=selb[g2], identity=k.ident16[:]),
                 reads=[('selb', g2), 'ident16'], writes=[('psb', 7)])
            S.op('act', lambda h, pt_=pt_, tr=tr, qt=qt: h.copy(out=tr[0:32, qt // 4, (qt % 4) * 128:(qt % 4 + 1) * 128], in_=pt_),
                 reads=[('psb', 7)], writes=[('TR', sl, qt // 4)])
        for qb in range(4):
            for kt in range(2):
                sb = cnt['s'] % 3
                cnt['s'] += 1
                ps = ps_bank(k, sb, 256)
                pi = cnt['p'] % 3
                cnt['p'] += 1
                pt = PT[pi]
                ktile = 2 * qb + kt
                S.op('pe', lambda h, ps=ps, ktile=ktile, hh=hh, qb=qb, kl16=kl16: h.matmul(
                    ps, kl16[:, ktile * 128:(ktile + 1) * 128], QT[:, hh, qb * 256:(qb + 1) * 256], start=True, stop=False),
                    reads=[('KL', sl), ('QT', hh, qb // 2)], writes=[('psb', sb)])
                S.op('pe', lambda h, ps=ps, kt=kt: h.matmul(ps, k.ident16[:], cmask[:, kt, :], start=False, stop=False),
                     reads=['ident16', 'cmask'], writes=[('psb', sb)])
                S.op('pe', lambda h, ps=ps, tr=tr: h.matmul(ps, TL[32:35, 0:128], tr[32:35, 0, 0:256], start=False, stop=True),
                     reads=['TL', ('TRs', sl)], writes=[('psb', sb)])
                S.op('act', lambda h, ps=ps, pt=pt, hh=hh, kt=kt: h.activation(
                    out=pt[:, 0:256], in_=ps, func=AF.Exp, bias=bown[:, hh * 2 + kt:hh * 2 + kt + 1], scale=MSC),
                    reads=[('psb', sb), 'bown'], writes=[('PT', pi)])
                for i in range(2):
                    qt = 2 * qb + i
                    a_ap, a_b = acc[qt]
                    S.op('pe', lambda h, a_ap=a_ap, pt=pt, i=i, vl16=vl16, ktile=ktile, kt=kt: h.matmul(
                        a_ap, pt[:, i * 128:(i + 1) * 128], vl16[:, ktile * 129:(ktile + 1) * 129],
                        start=(kt == 0 and i == 0), stop=False),
                        reads=[('PT', pi), ('VL', sl)], writes=[('psb', a_b)])
        for ch in range(4):
            ks = cnt['kv'] % 3
            cnt['kv'] += 1
            kc = Kc[ks]
            vc = Vc[ks]
            S.dma('sp', lambda h, kc=kc, ch=ch, hh=hh: h.dma_start(
                out=kc.rearrange("p (r c) -> p r c", r=2), in_=k.kv_allh(hh)[0][:, 2 * ch:2 * ch + 2, :]),
                reads=[('kvall', hh)], writes=[('Kc', ks)])
            S.dma('sp', lambda h, vc=vc, ch=ch, hh=hh: h.dma_start(
                out=vc.rearrange("p (r c) -> p r c", r=2),
                in_=k.kv_allh(hh)[1][:, 2 * ch:2 * ch + 2, :]),
                reads=[('kvall', hh)], writes=[('Vc', ks)])
            kc16 = kc.bitcast(BF16)
            vc16 = vc.bitcast(BF16)
            for half in range(2):
                for mt in range(16):
                    m = ch * 16 + mt
                    n = m // 2
                    sb = cnt['s'] % 3
                    cnt['s'] += 1
                    ps = ps_bank(k, sb)
                    pi = cnt['p'] % 3
                    cnt['p'] += 1
                    pt = PT[pi]
                    S.op('pe', lambda h, ps=ps, kc16=kc16, mt=mt, hh=hh, half=half: h.matmul(
                        ps, kc16[:, mt * 128:(mt + 1) * 128], QT[:, hh, half * 512:(half + 1) * 512], start=True, stop=False),
                        reads=[('Kc', ks), ('QT', hh, half)], writes=[('psb', sb)])
                    S.op('pe', lambda h, ps=ps, n=n, tr=tr, half=half: h.matmul(
                        ps, TL[:, n * 128:(n + 1) * 128], tr[:, half, :], start=False, stop=True),
                        reads=['TL', ('TRs', sl), ('TR', sl, half), ('TRz', id(tr))], writes=[('psb', sb)])
                    S.op('act', lambda h, ps=ps, pt=pt, bt=bt, m=m, half=half: h.activation(
                        out=pt, in_=ps, func=AF.Exp, bias=bt[:, m * 2 + half:m * 2 + half + 1], scale=MSC),
                        reads=[('psb', sb), ('bt', sl)], writes=[('PT', pi)])
                    def emit_pv(pt=pt, pi=pi, vc16=vc16, mt=mt, m=m, half=half, ks=ks):
                        for i in range(4):
                            qt = half * 4 + i
                            a_ap, a_b = acc[qt]
                            S.op('pe', lambda h, a_ap=a_ap, pt=pt, i=i, vc16=vc16, mt=mt, m=m: h.matmul(
                                a_ap, pt[:, i * 128:(i + 1) * 128],
                                vc16[:, (mt // 8) * 1032 + (mt % 8) * 129:(mt // 8) * 1032 + (mt % 8) * 129 + 129],
                                start=False, stop=(m == 63)),
                                reads=[('PT', pi), ('Vc', ks)], writes=[('psb', a_b)])
                    if pend_pv:
                        pend_pv.pop(0)()
                    pend_pv.append(emit_pv)
        while pend_pv:
            pend_pv.pop(0)()
        for qt in range(NT):
            a_ap, a_b = acc[qt]
            oi = cnt['o'] % 2
            ri = cnt['o'] % 4
            cnt['o'] += 1
            S.op('dve', lambda h, a_ap=a_ap, ri=ri: h.reciprocal(out=rden[ri], in_=a_ap[:, 128:129]),
                 reads=[('psb', a_b)], writes=[('rden', ri)])
            S.op('dve', lambda h, a_ap=a_ap, ri=ri, oi=oi: h.tensor_scalar(
                out=On[oi], in0=a_ap[:, 0:128], scalar1=rden[ri][:, 0:1], scalar2=None, op0=ALU.mult),
                reads=[('psb', a_b), ('rden', ri)], writes=[('On', oi)])
            ti = cnt['t'] % 6
            cnt['t'] += 1
            pt_ = T7[:, ti * 128:(ti + 1) * 128]
            S.op('pe', lambda h, pt_=pt_, oi=oi: h.transpose(out=pt_, in_=On[oi], identity=k.ident16[:]),
                 reads=[('On', oi), 'ident16'], writes=[('psb', 7)])
            S.op('act', lambda h, pt_=pt_, hh=hh, qt=qt: h.copy(out=k.hT[:, hh, qt * 128:(qt + 1) * 128], in_=pt_),
                 reads=[('psb', 7)], writes=[('h', hh, qt // 4)])
    S.barrier()
    C2 = Carver(k.R, k.RN)
    proj_out(k, Wo, C2)
    S.barrier()


def final_norm_out(k):
    S = k.S
    yo = k.dout("y", [TC, D])
    C = Carver(k.R, k.RN)
    sq = [C.bf16(512) for _ in range(2)]
    rstd = [C.f32(512) for _ in range(2)]
    yst = [C.f32(D) for _ in range(2)]
    S.op('dve', lambda h: h.tensor_scalar(out=k.colA[:], in0=k.fgT[:], scalar1=float(np.sqrt(D)), scalar2=None,
                                          op0=ALU.mult), reads=['fgT'], writes=['colA'])
    for hf in range(2):
        ts = slice(hf * 512, (hf + 1) * 512)
        psq = ps_bank(k, 4 + hf)
        for j in range(NJ):
            q = sq[j % 2]
            S.op('act', lambda h, q=q, j=j, ts=ts: h.activation(out=q, in_=k.xT[:, j, ts], func=AF.Square),
                 reads=[('x', j, hf)], writes=[('sq', j % 2)])
            S.op('pe', lambda h, q=q, j=j, psq=psq: h.matmul(psq, k.ones16[:], q, start=(j == 0), stop=(j == NJ - 1)),
                 reads=[('sq', j % 2), 'ones16'], writes=[('psb', 4 + hf)])
        r = rstd[hf]
        S.op('act', lambda h, r=r, psq=psq: h.activation(out=r, in_=psq, func=AF.Sqrt, bias=k.epsc[:, 0:1], scale=1.0),
             reads=[('psb', 4 + hf), 'epsc'], writes=[('rstd', hf)])
        S.op('dve', lambda h, r=r: h.reciprocal(out=r, in_=r), reads=[('rstd', hf)], writes=[('rstd', hf)])
        for j in range(NJ):
            S.op('dve', lambda h, j=j, r=r, ts=ts: h.scalar_tensor_tensor(
                out=k.xT[:, j, ts], in0=k.xT[:, j, ts], scalar=k.colA[:, j:j + 1], in1=r,
                op0=ALU.mult, op1=ALU.mult),
                reads=[('x', j, hf), ('rstd', hf), 'colA'], writes=[('x', j, hf)])
    for tt in range(TC // 128):
        ys = yst[tt % 2]
        hf = tt // 4
        for jg in range(NJ // 4):
            b = jg % 4
            pst = ps_bank(k, b).rearrange("p (a t) -> p a t", a=4)
            for a in range(4):
                j = jg * 4 + a
                S.op('pe', lambda h, j=j, a=a, pst=pst, tt=tt: h.transpose(
                    out=pst[:, a, :], in_=k.xT[:, j, tt * 128:(tt + 1) * 128], identity=k.ident32[:]),
                    reads=[('x', j, hf), 'ident32'], writes=[('psb', b)])
            dst = ys[:, jg * 512:(jg + 1) * 512]
            if jg % 2 == 0:
                S.op('dve', lambda h, dst=dst, b=b: h.tensor_copy(out=dst, in_=ps_bank(k, b)),
                     reads=[('psb', b)], writes=[('yst', tt % 2, jg)])
            else:
                S.op('act', lambda h, dst=dst, b=b: h.copy(out=dst, in_=ps_bank(k, b)),
                     reads=[('psb', b)], writes=[('yst', tt % 2, jg)])
        S.dma('sp', lambda h, ys=ys, tt=tt: h.dma_start(out=yo[tt * 128:(tt + 1) * 128, :], in_=ys),
              reads=[('yst', tt % 2, jg) for jg in range(4)], writes=[('out', tt)])


class HostData:
    def __init__(self, inp):
        f = lambda a: np.ascontiguousarray(np.asarray(a, dtype=np.float32))
        self.inp = inp
        self.f = f
        bf = ml_dtypes.bfloat16
        self.common = {
            'cvec': f(inp['c']).reshape(NJ, 128),
            'norm_g': f(inp['norm_g']).reshape(DEPTH * 3 * NJ, 128),
            'final_g': f(inp['final_g']).reshape(NJ, 128),
            'pool_scale': f(inp['pool_scale']).reshape(NJ, 128),
            'ident': np.eye(128, dtype=np.float32),
            'pool_w': f(inp['pool_w'])[0],
            'swa_w_qkv': f(inp['swa_w_qkv'])[0],
            'swa_w_o': f(inp['swa_w_o'])[0],
        }
        self.x = f(inp['x'])[0]
        self.ada_b = f(inp['ada_b']).reshape(DEPTH * NMODC, 128)
        sl32 = np.array([2.0 ** (-8.0 * (i + 1) / 32) for i in range(32)], np.float32)
        p = np.arange(128, dtype=np.float32)
        self.sl32, self.p = sl32, p

        def split3(v, axis):
            h_ = v.astype(bf); r_ = (v - h_.astype(np.float32)).astype(np.float32)
            m_ = r_.astype(bf); l_ = (r_ - m_.astype(np.float32)).astype(bf)
            return np.stack([h_, m_, l_], axis)
        vq = (-(sl32[:, None] * p[None, :]) / np.float32(0.125)).astype(np.float32)
        self.common['swa_slq'] = np.ascontiguousarray(split3(vq, 0))
        NEGM = np.float32(-240000.0)
        kk = np.arange(128)[:, None]; qq = np.arange(128)[None, :]
        self.cprev = np.where(kk > qq, 0.0, NEGM).astype(np.float32)
        self.cown = np.where(kk <= qq, 0.0, NEGM).astype(np.float32)
        self.NEGM = NEGM
        self.sinks = f(inp['swa_sinks'])[0]
        sl16 = np.array([2.0 ** (-8.0 * (i + 1) / 16) for i in range(16)], np.float32)
        self.sl16 = sl16
        q512 = np.arange(512, dtype=np.float32)
        vq16 = (-(sl16[:, None] * q512[None, :]) / np.float32(MSC)).astype(np.float32)
        sp3 = split3(vq16, 1)
        self.common['moba_slq'] = np.ascontiguousarray(np.stack([sp3, sp3], 2))
        TLm = np.zeros((35, 32, 128), np.float32)
        for n_ in range(32):
            TLm[n_, n_, :] = 1.0
        TLm[32:35] = 1.0
        self.common['moba_TL'] = np.ascontiguousarray(TLm.reshape(35, 4096).astype(bf))
        cmk = np.zeros((128, 2, 256), np.float32)
        for kt_ in range(2):
            cmk[:, kt_, :] = np.where((kt_ * 128 + p[:, None]) <= np.arange(256)[None, :], 0.0, -BIG)
        self.common['moba_cmask'] = np.ascontiguousarray(cmk.astype(bf))
        bown = np.zeros((128, 16, 2), np.float32)
        for kt_ in range(2):
            bown[:, :, kt_] = sl16[None, :] * (kt_ * 128 + p[:, None])
        self.common['moba_bown'] = np.ascontiguousarray(bown.reshape(128, 32))

    def get(self, name, c):
        inp, f, p = self.inp, self.f, self.p
        bf = ml_dtypes.bfloat16
        if name in self.common:
            return self.common[name]
        if name == 'x_c':
            return np.ascontiguousarray(self.x[c * TC:(c + 1) * TC])
        if name == 'ada_w_c':
            half = MODPC * 128
            return np.ascontiguousarray(np.asarray(inp['ada_w'][c // 2], dtype=np.float32)[:, (c % 2) * half:(c % 2 + 1) * half])
        if name == 'ada_b_c':
            return np.ascontiguousarray(self.ada_b[c * MODPC:(c + 1) * MODPC])
        if name[0] == 'w' and name[1] in 'gud' and name[2] == '_':
            _, l, s_ = name.split('_')
            key = {'g': 'ffn_w_gate', 'u': 'ffn_w_up', 'd': 'ffn_w_down'}[name[1]]
            return f(inp[key][int(l), int(s_)])
        if name.startswith('moba_qkv_'):
            return f(inp['moba_w_qkv'][int(name.split('_')[-1])])
        if name.startswith('moba_o_'):
            return f(inp['moba_w_o'][int(name.split('_')[-1])])
        if name == 'onehot_prev':
            oh = np.zeros((128, NCORES), np.float32)
            if c > 0:
                oh[:, c - 1] = 1.0
            return oh
        if name == 'pool_invc':
            inv = np.zeros((4, 16), np.float32)
            for L in range(4):
                w = 2 ** (L + 1)
                for t in range(16):
                    inv[L, t] = 1.0 / (min(t + 1, w) if c == 0 else w)
            return np.ascontiguousarray(np.broadcast_to(inv.reshape(1, 64), (128, 64)))
        if name == 'moba_pastb':
            pb = np.zeros((4, 32), np.float32)
            for qb_ in range(4):
                pb[qb_, :] = np.where(np.arange(32) < 4 * c + qb_, 0.0, -1e30)
            return np.ascontiguousarray(np.broadcast_to(pb.reshape(1, 128), (128, 128)))
        if name == 'moba_bias':
            mb = np.zeros((16, 128, 64, 2), np.float32)
            mm_ = np.arange(64, dtype=np.float32)
            for half_ in range(2):
                pos = 128.0 * mm_[None, :] + p[:, None] - (1024.0 * c + 512.0 * half_)
                mb[:, :, :, half_] = self.sl16[:, None, None] * pos[None, :, :]
            return np.ascontiguousarray(mb.reshape(16, 128, 128))
        if name == 'swa_tab':
            tab = np.zeros((128, 96), np.float32)
            tab[:, 0:32] = self.sl32[None, :] * (p[:, None] - 128.0)
            tab[:, 32:64] = self.sl32[None, :] * p[:, None]
            tab[:, 64:96] = self.sinks[None, :]
            return tab
        if name == 'swa_cm':
            c0 = self.cprev if c > 0 else np.full((128, 128), self.NEGM, np.float32)
            return np.ascontiguousarray(np.stack([self.cprev, self.cown, c0], 1).astype(bf))
        raise KeyError(name)


def run_segments(inputs, cfg_base, nseg=None):
    H = HostData(inputs)
    state = None
    ex = None
    res = None
    segs = range(len(SEGMENTS)) if nseg is None else range(nseg)
    for seg in segs:
        cfg = dict(cfg_base)
        cfg['seg'] = seg
        nc, k = build_program(cfg)
        maps = []
        ex_all = None
        if 'ex_all' in k.used_inputs:
            ex_all = np.ascontiguousarray(np.concatenate(ex, axis=0))
        for c in range(NCORES):
            m = {}
            for name in k.used_inputs:
                if name == 'state_in':
                    m[name] = state[c]
                elif name == 'ex_all':
                    m[name] = ex_all
                elif name == 'ex_own':
                    m[name] = ex[c]
                else:
                    m[name] = H.get(name, c)
            maps.append(m)
        res = run_bass_kernel_spmd(nc, maps, core_ids=list(range(NCORES))).results
        if 'state_out' in k.outputs:
            state = [np.asarray(r['state_out']) for r in res]
        if 'ex_out' in k.outputs:
            ex = [np.asarray(r['ex_out']) for r in res]
    return res


def run_fused(inputs, cfg_base):
    H = HostData(inputs)
    cfg = dict(cfg_base)
    cfg['fused'] = True
    nc, k = build_program(cfg)
    maps = [{name: H.get(name, c) for name in k.used_inputs} for c in range(NCORES)]
    return run_bass_kernel_spmd(nc, maps, core_ids=list(range(NCORES))).results


FUSED = True


def kernel(**inputs):
    res = run_fused(inputs, {}) if FUSED else run_segments(inputs, {})
    y = np.concatenate([np.asarray(r['y']) for r in res], axis=0)
    return y[None].astype(np.float32)
```

```python
import numpy as np
import ml_dtypes
from contextlib import ExitStack

import concourse.bass as bass
import concourse.mybir as mybir
from concourse.bass_utils import run_bass_kernel_spmd

F32 = mybir.dt.float32
BF16 = mybir.dt.bfloat16
AF = mybir.ActivationFunctionType
ALU = mybir.AluOpType
AX = mybir.AxisListType

NCORES = 8
D = 2048
T = 8192
TC = T // NCORES
NJ = D // 128
FF = 5632
NFC = FF // 128
DEPTH = 4
EPS = 1e-6
NMODC = 9 * NJ
MODPC = DEPTH * NMODC // NCORES
BIG = 339411.0

ENGS = ('pe', 'act', 'dve', 'pool', 'sp')
SYNC_SAME = {'pe': False, 'act': True, 'dve': True, 'pool': True, 'sp': False}


class _Op:
    __slots__ = ('eng', 'fn', 'deps', 'dma', 'marked', 'val')

    def __init__(self, eng, fn, deps, dma=None):
        self.eng = eng
        self.fn = fn
        self.deps = deps
        self.dma = dma
        self.marked = False
        self.val = 0


class Sched:
    def __init__(self, n_dma_sems):
        self.ops = {e: [] for e in ENGS}
        self.lastw = {}
        self.readers = {}
        self.nd = n_dma_sems
        self.dcnt = [0] * n_dma_sems
        self.drr = 0
        self.outstanding = []

    def _deps(self, reads, writes):
        deps = []
        for r in reads:
            t = self.lastw.get(r)
            if t is not None:
                deps.append(t)
        for w in writes:
            t = self.lastw.get(w)
            if t is not None:
                deps.append(t)
            deps.extend(self.readers.get(w, ()))
        return deps

    def _commit(self, tok, reads, writes):
        for r in reads:
            lst = self.readers.setdefault(r, [])
            if tok[0] == 'e':
                for i, t in enumerate(lst):
                    if t[0] == 'e' and t[1] == tok[1]:
                        lst[i] = tok
                        break
                else:
                    lst.append(tok)
            else:
                lst.append(tok)
        for w in writes:
            self.lastw[w] = tok
            self.readers[w] = []

    def op(self, eng, fn, reads=(), writes=()):
        deps = self._deps(reads, writes)
        idx = len(self.ops[eng])
        self.ops[eng].append(_Op(eng, fn, deps))
        self._commit(('e', eng, idx), reads, writes)

    def dma(self, eng, fn, reads=(), writes=(), inc=16):
        k = self.drr
        self.drr = (k + 1) % self.nd
        prev = self.dcnt[k]
        self.dcnt[k] += inc
        val = self.dcnt[k]
        deps = self._deps(reads, writes)
        if prev > 0:
            deps.append(('d', k, prev))
        self.ops[eng].append(_Op(eng, fn, deps, dma=(k, val, inc)))
        tok = ('d', k, val)
        self.outstanding.append(tok)
        self._commit(tok, reads, writes)

    def barrier(self):
        toks = []
        for e in ENGS:
            for i in range(len(self.ops[e]) - 1, -1, -1):
                if self.ops[e][i].dma is None:
                    toks.append(('e', e, i))
                    break
        toks += self.outstanding
        for e in ENGS:
            self.ops[e].append(_Op(e, None, list(toks)))
        self.outstanding = []
        self.lastw = {}
        self.readers = {}

    def emit(self, block, sems, dsems):
        for e in ENGS:
            for o in self.ops[e]:
                for t in o.deps:
                    if t[0] == 'e' and (t[1] != e or SYNC_SAME[e]):
                        self.ops[t[1]][t[2]].marked = True
        for e in ENGS:
            c = 0
            for o in self.ops[e]:
                if o.marked:
                    c += 1
                o.val = c
        handles = {'pe': block.tensor, 'act': block.scalar, 'dve': block.vector,
                   'pool': block.gpsimd, 'sp': block.sync}
        for e in ENGS:
            ops = self.ops[e]
            if not ops:
                continue

            def body(h, e=e, ops=ops):
                waited_e = {}
                waited_d = {}
                for o in ops:
                    need_e = {}
                    need_d = {}
                    for t in o.deps:
                        if t[0] == 'e':
                            if t[1] == e and not SYNC_SAME[e]:
                                continue
                            if t[2] > need_e.get(t[1], -1):
                                need_e[t[1]] = t[2]
                        else:
                            if t[2] > need_d.get(t[1], 0):
                                need_d[t[1]] = t[2]
                    for e2, i2 in need_e.items():
                        if i2 > waited_e.get(e2, -1):
                            h.wait_ge(sems[e2], self.ops[e2][i2].val)
                            waited_e[e2] = i2
                    for k, v in need_d.items():
                        if v > waited_d.get(k, 0):
                            h.wait_ge(dsems[k], v)
                            waited_d[k] = v
                    if o.fn is None:
                        ins = h.nop() if o.marked else None
                    else:
                        ins = o.fn(h)
                    if o.dma is not None:
                        ins.then_inc(dsems[o.dma[0]], o.dma[2])
                    elif o.marked:
                        ins.then_inc(sems[e], 1)
            handles[e](body)


class Carver:
    def __init__(self, region, n):
        self.r = region
        self.n = n
        self.off = 0

    def f32(self, n):
        a = self.r[:, self.off:self.off + n]
        self.off += n
        assert self.off <= self.n, (self.off, self.n)
        return a

    def bf16(self, n):
        m = (n + 1) // 2
        a = self.r[:, self.off:self.off + m].bitcast(BF16)
        self.off += m
        assert self.off <= self.n, (self.off, self.n)
        return a

    def mark(self):
        return self.off

    def reset(self, off):
        self.off = off


INSPEC = {
    'x_in': ("x_c", [TC, D], F32),
    'cvec': ("cvec", [NJ, 128], F32),
    'normg': ("norm_g", [DEPTH * 3 * NJ, 128], F32),
    'finalg': ("final_g", [NJ, 128], F32),
    'pscale': ("pool_scale", [NJ, 128], F32),
    'adaw': ("ada_w_c", [D, MODPC * 128], F32),
    'adab': ("ada_b_c", [MODPC, 128], F32),
    'ident_in': ("ident", [128, 128], F32),
    'onehot_in': ("onehot_prev", [128, NCORES], F32),
    'pinvc_in': ("pool_invc", [128, 64], F32),
    'pool_w': ("pool_w", [4, 512, 512], F32),
    'swa_qkv': ("swa_w_qkv", [D, 2560], F32),
    'swa_o': ("swa_w_o", [D, D], F32),
    'swa_tab': ("swa_tab", [128, 96], F32),
    'swa_slq': ("swa_slq", [3, 32, 128], BF16),
    'swa_cm': ("swa_cm", [128, 3, 128], BF16),
    'moba_pastb': ("moba_pastb", [128, 128], F32),
    'moba_TL': ("moba_TL", [35, 32 * 128], BF16),
    'moba_slq': ("moba_slq", [16, 3, 2, 512], BF16),
    'moba_bias': ("moba_bias", [16, 128, 128], F32),
    'moba_cmask': ("moba_cmask", [128, 2, 256], BF16),
    'moba_bown': ("moba_bown", [128, 32], F32),
}
NTOT = 16384 + 8192 + 576 + 192 + 16 + 16 + 128 + 64 + 64 + 16 + 16 + 16 + 2 + 8 + 64 + 26000


class K:
    def __init__(self, nc, cfg):
        self.__dict__['_decl'] = {}
        self.nc = nc
        self.cfg = cfg
        self.fused = cfg.get('fused', False)
        self.used_inputs = []
        self.outputs = []

    def din(self, name, shape, dt=F32):
        if name not in self._decl:
            self._decl[name] = self.nc.dram_tensor(name, list(shape), dt, kind="ExternalInput").ap()
            self.used_inputs.append(name)
        return self._decl[name]

    def dout(self, name, shape, dt=F32):
        if name not in self._decl:
            self._decl[name] = self.nc.dram_tensor(name, list(shape), dt, kind="ExternalOutput").ap()
            self.outputs.append(name)
        return self._decl[name]

    def dint(self, name, shape, dt=F32):
        if name not in self._decl:
            self._decl[name] = self.nc.dram_tensor(name, list(shape), dt, kind="Internal").ap()
        return self._decl[name]

    def __getattr__(self, name):
        if name in INSPEC:
            nm, shp, dt = INSPEC[name]
            return self.din(nm, shp, dt)
        raise AttributeError(name)

    def ffnw(self, which, l, s):
        shp = [FF, D] if which == 'd' else [D, FF]
        return self.din("w%s_%d_%d" % (which, l, s), shp)

    def mobaw(self, which, jm):
        return self.din("moba_%s_%d" % (which, jm), [D, 3 * D] if which == 'qkv' else [D, D])

    def ex_w(self, tag, width):
        if self.fused:
            return self.dint("loc_" + tag, [128, width])
        return self.dout("ex_out", [128, width])

    def ex_all(self, tag, width):
        if self.fused:
            return self.dint("all_" + tag, [NCORES * 128, width])
        return self.din("ex_all", [NCORES * 128, width])

    def ex_own(self, tag, width):
        if self.fused:
            return self.dint("loc_" + tag, [128, width])
        return self.din("ex_own", [128, width])

    def kv_w(self, hh):
        if self.fused:
            t = self.dint("loc_kv%d" % hh, [128, 1028])
            return t[:, 0:512], t[:, 512:1028]
        t = self.ex_w('kv', KVW)
        return t[:, hh * 512:(hh + 1) * 512], t[:, 8192 + hh * 516:8192 + (hh + 1) * 516]

    def kv_own(self, hh):
        if self.fused:
            return self.kv_w(hh)
        t = self.ex_own('kv', KVW)
        return t[:, hh * 512:(hh + 1) * 512], t[:, 8192 + hh * 516:8192 + (hh + 1) * 516]

    def kv_allh(self, hh):
        if self.fused:
            t = self.dint("all_kv%d" % hh, [NCORES * 128, 1028]).rearrange("(r p) c -> p r c", p=128)
            return t[:, :, 0:512], t[:, :, 512:1028]
        t = self.ex_all('kv', KVW).rearrange("(r p) c -> p r c", p=128)
        return t[:, :, hh * 512:(hh + 1) * 512], t[:, :, 8192 + hh * 516:8192 + (hh + 1) * 516]

    def km_w(self):
        if self.fused:
            return self.dint("loc_km", [128, 64])
        return self.ex_w('kv', KVW)[:, 16448:16512]

    def km_all(self):
        if self.fused:
            return self.dint("all_km", [NCORES * 128, 64]).rearrange("(r p) c -> p r c", p=128)
        return self.ex_all('kv', KVW).rearrange("(r p) c -> p r c", p=128)[:, :, 16448:16512]

    def exchange(self, tag, width, reads, outkey):
        if not self.fused:
            return
        src = self.ex_w(tag, width)
        dst = self.ex_all(tag, width)
        self.S.dma('pool', lambda h: h.collective_compute(
            "AllGather", ALU.bypass, replica_groups=[list(range(NCORES))],
            ins=[src.opt()], outs=[dst.opt()]), reads=reads, writes=[outkey], inc=1)


SEGMENTS = [
    [('pro1',)],
    [('pro2',), ('ffn', 0, 0), ('mobaA', 0)],
    [('mobaB', 0), ('ffn', 0, 1), ('ffn', 1, 0), ('poolA', 1)],
    [('poolB', 1), ('ffn', 1, 1), ('ffn', 2, 0), ('swaA', 2)],
    [('swaB', 2), ('ffn', 2, 1), ('ffn', 3, 0), ('mobaA', 3)],
    [('mobaB', 3), ('ffn', 3, 1), ('final',)],
]


def build_program(cfg):
    seg = cfg.get('seg', None)
    nc = bass.Bass("TRN2", target_bir_lowering=False, num_devices=NCORES)
    k = K(nc, cfg)
    if seg is None:
        stages = cfg.get('stages') or [st for sg in SEGMENTS for st in sg]
        first, last = True, True
    else:
        stages = SEGMENTS[seg]
        first, last = seg == 0, seg == len(SEGMENTS) - 1
    dbg_x = cfg.get('dbg_x', False)
    with ExitStack() as es:
        SB = es.enter_context(nc.sbuf_tensor("SB", [128, NTOT], F32))
        k.SB = SB
        st = {'o': 0}

        def cv(n):
            a_ = SB[:, st['o']:st['o'] + n]
            st['o'] += n
            return a_
        k.xT = cv(NJ * TC).rearrange("p (j t) -> p j t", j=NJ)
        k.hT = cv(NJ * TC // 2).bitcast(BF16).rearrange("p (j t) -> p j t", j=NJ)
        k.modT = cv(DEPTH * NMODC)
        k.gT = cv(DEPTH * 3 * NJ)
        k.fgT = cv(NJ)
        k.psT = cv(NJ)
        k.ident32 = cv(128)
        k.ident16 = cv(64).bitcast(BF16)
        k.ones16 = cv(64).bitcast(BF16)
        k.colA = cv(NJ)
        k.colG = cv(NJ)
        k.cs32 = cv(NJ)
        k.epsc = cv(2)
        k.onehot = cv(NCORES)
        k.pinvc = cv(64)
        RN = 26000
        k.RN = RN
        k.R = cv(RN)
        assert st['o'] == NTOT
        k.PS = es.enter_context(nc.psum_tensor("PS", [128, 8 * 512], F32))
        sems = {e: es.enter_context(nc.semaphore("s_" + e)) for e in ENGS}
        ND = 24
        dsems = [es.enter_context(nc.semaphore("d%d" % i)) for i in range(ND)]
        block = es.enter_context(nc.Block())
        S = Sched(ND)
        k.S = S
        CH = 8192
        nch = (NTOT + CH - 1) // CH
        if not first:
            sin = k.din("state_in", [128, NTOT])
            for i in range(nch):
                lo, hi = i * CH, min(NTOT, (i + 1) * CH)
                S.dma('sp', lambda h, lo=lo, hi=hi: h.dma_start(out=SB[:, lo:hi], in_=sin[:, lo:hi]), writes=[('st', i)])
            S.barrier()
        for stg in stages:
            nm = stg[0]
            if nm == 'pro1':
                prologue1(k)
            elif nm == 'pro2':
                prologue2(k)
            elif nm == 'ffn':
                norm_mod(k, stg[1], 0 if stg[2] == 0 else 2)
                ffn(k, stg[1], stg[2])
            elif nm == 'mobaA':
                moba_A(k, stg[1], cfg.get('jm', stg[1] // 3))
            elif nm == 'mobaB':
                moba_B(k, stg[1], cfg.get('jm', stg[1] // 3))
            elif nm == 'poolA':
                pool_A(k, stg[1])
            elif nm == 'poolB':
                pool_B(k, stg[1])
            elif nm == 'swaA':
                swa_A(k, stg[1])
            elif nm == 'swaB':
                swa_B(k, stg[1])
            elif nm == 'final':
                S.barrier()
                if dbg_x:
                    xo = k.dout("xT_out", [128, NJ * TC])
                    S.dma('sp', lambda h: h.dma_start(out=xo.rearrange("p (j t) -> p j t", j=NJ), in_=k.xT), writes=['out'])
                else:
                    final_norm_out(k)
        fin = ['out'] + [('out', tt) for tt in range(8)] + ['exo']
        if not last:
            S.barrier()
            sout = k.dout("state_out", [128, NTOT])
            for i in range(nch):
                lo, hi = i * CH, min(NTOT, (i + 1) * CH)
                S.dma('sp', lambda h, lo=lo, hi=hi: h.dma_start(out=sout[:, lo:hi], in_=SB[:, lo:hi]), writes=[('sto', i)])
                fin.append(('sto', i))
        S.op('sp', lambda h: h.nop(), reads=fin)
        S.emit(block, sems, dsems)
    return nc, k


def ps_bank(k, b, n=512):
    return k.PS[:, b * 512:b * 512 + n]


def xkeys(hf=None):
    if hf is None:
        return [('x', j, h) for j in range(NJ) for h in range(2)]
    return [('x', j, hf) for j in range(NJ)]


def prologue1(k):
    S = k.S
    C = Carver(k.R, k.RN)
    S.dma('sp', lambda h: h.dma_start(out=k.ident32[:], in_=k.ident_in), writes=['ident32'])
    S.op('dve', lambda h: h.tensor_copy(out=k.ident16[:], in_=k.ident32[:]), reads=['ident32'], writes=['ident16'])
    S.op('dve', lambda h: h.memset(k.ones16[:], 1.0), writes=['ones16'])
    S.op('dve', lambda h: h.memset(k.epsc[:], float(D * EPS)), writes=['epsc'])
    S.dma('sp', lambda h: h.dma_start(out=k.onehot[:], in_=k.onehot_in), writes=['onehot'])
    S.dma('sp', lambda h: h.dma_start(out=k.pinvc[:], in_=k.pinvc_in), writes=['pinvc'])

    rows = C.f32(128)
    def to_cols(src_ap, nrows, dst_ap, tag, func=None):
        S.dma('sp', lambda h: h.dma_start(out=rows[0:nrows, :], in_=src_ap), writes=['rows'])
        pst = ps_bank(k, 7, nrows)
        S.op('pe', lambda h: h.transpose(out=pst, in_=rows[0:nrows, :], identity=k.ident32[0:nrows, 0:nrows]),
             reads=['rows', 'ident32'], writes=[('psb', 7)])
        if func is None:
            S.op('dve', lambda h: h.tensor_copy(out=dst_ap, in_=pst), reads=[('psb', 7)], writes=[tag])
        else:
            S.op('act', lambda h: h.activation(out=dst_ap, in_=pst, func=func), reads=[('psb', 7)], writes=[tag])
    to_cols(k.cvec, NJ, k.cs32[:], 'cs32', AF.Silu)
    to_cols(k.normg[0:96, :], 96, k.gT[:, 0:96], 'gT0')
    to_cols(k.normg[96:192, :], 96, k.gT[:, 96:192], 'gT1')
    to_cols(k.finalg, NJ, k.fgT[:], 'fgT')
    to_cols(k.pscale, NJ, k.psT[:], 'psT')
    adabT = C.f32(MODPC)
    to_cols(k.adab, MODPC, adabT, 'adabT')

    NG = 4
    nslab = MODPC // NG
    wslab = [C.f32(NJ * NG * 128).rearrange("p (j f) -> p j f", j=NJ) for _ in range(2)]
    psm = ps_bank(k, 6, MODPC)
    for sidx in range(nslab):
        w = wslab[sidx % 2]
        src = k.adaw[:, sidx * NG * 128:(sidx + 1) * NG * 128].rearrange("(j p) f -> p j f", p=128)
        S.dma('sp', lambda h, w=w, src=src: h.dma_start(out=w, in_=src), writes=[('adaw', sidx % 2)])
        for g in range(NG):
            jj = sidx * NG + g
            for kc in range(NJ):
                S.op('pe', lambda h, w=w, g=g, jj=jj, kc=kc: h.matmul(
                    psm[:, jj:jj + 1], w[:, kc, g * 128:(g + 1) * 128], k.cs32[:, kc:kc + 1],
                    start=(kc == 0), stop=(kc == NJ - 1)),
                    reads=[('adaw', sidx % 2), 'cs32'], writes=[('psb', 6)])
    modloc = C.f32(MODPC)
    S.op('dve', lambda h: h.tensor_tensor(out=modloc, in0=psm, in1=adabT, op=ALU.add),
         reads=[('psb', 6), 'adabT'], writes=['modloc'])
    mloc = k.ex_w('mod', MODPC)
    S.dma('pool', lambda h: h.dma_start(out=mloc, in_=modloc), reads=['modloc'], writes=['exo'])
    k.exchange('mod', MODPC, ['exo'], 'mod_all_d')
    S.barrier()


def prologue2(k):
    S = k.S
    C = Carver(k.R, k.RN)
    mall = k.ex_all('mod', MODPC)
    S.dma('pool', lambda h: h.dma_start(out=k.modT.rearrange("p (r c) -> p r c", r=NCORES),
                                        in_=mall.rearrange("(r p) c -> p r c", p=128)),
          reads=['mod_all_d'], writes=['modT'])

    xst = [C.f32(D) for _ in range(2)]
    for tt in range(TC // 128):
        xs = xst[tt % 2]
        S.dma('sp', lambda h, xs=xs, tt=tt: h.dma_start(out=xs, in_=k.x_in[tt * 128:(tt + 1) * 128, :]),
              writes=[('xst', tt % 2)])
        for jg in range(NJ // 4):
            b = jg % 4
            pst = ps_bank(k, b).rearrange("p (a t) -> p a t", a=4)
            for a in range(4):
                j = jg * 4 + a
                S.op('pe', lambda h, xs=xs, j=j, a=a, pst=pst: h.transpose(
                    out=pst[:, a, :], in_=xs[:, j * 128:(j + 1) * 128], identity=k.ident32[:]),
                    reads=[('xst', tt % 2), 'ident32'], writes=[('psb', b)])
            dst = k.xT[:, jg * 4:(jg + 1) * 4, tt * 128:(tt + 1) * 128]
            eng = 'dve' if jg % 2 == 0 else 'act'
            if eng == 'dve':
                S.op('dve', lambda h, dst=dst, pst=pst: h.tensor_copy(out=dst, in_=pst),
                     reads=[('psb', b)], writes=[('x', j, tt // 4) for j in range(jg * 4, jg * 4 + 4)])
            else:
                S.op('act', lambda h, dst=dst, pst=pst: h.copy(out=dst, in_=pst),
                     reads=[('psb', b)], writes=[('x', j, tt // 4) for j in range(jg * 4, jg * 4 + 4)])
    S.barrier()


def norm_mod(k, l, s):
    norm_to_hT(k, l, s, None)


def ffn(k, l, s):
    S = k.S
    C = Carver(k.R, k.RN)
    sub = 0 if s == 0 else 2
    gb = l * NMODC + sub * 3 * NJ + 2 * NJ
    gsrc = k.modT[:, gb:gb + NJ]
    S.op('dve', lambda h: h.tensor_scalar(out=k.colG[:], in0=gsrc, scalar1=0.5, scalar2=None, op0=ALU.mult),
         reads=['modT'], writes=['colG'])
    GS = 4
    NG = NFC // GS
    NGU = 2
    act = [C.bf16(GS * TC).rearrange("p (c t) -> p c t", c=GS) for _ in range(2)]
    wgu = [(C.bf16(NJ * 256).rearrange("p (j f) -> p j f", j=NJ),
            C.bf16(NJ * 256).rearrange("p (j f) -> p j f", j=NJ)) for _ in range(NGU)]
    wdr = [C.bf16(D) for _ in range(2 * GS)]
    sil = [C.f32(512) for _ in range(2)]
    Wg = k.ffnw('g', l, s)
    Wu = k.ffnw('u', l, s)
    Wd = k.ffnw('d', l, s)
    st = {'pair': 0, 'evac': 0}

    def gateup(g):
        ab = act[g % 2]
        for pr in range(GS // 2):
            slot = st['pair'] % NGU
            st['pair'] += 1
            wgs, wus = wgu[slot]
            c0 = (g * GS + pr * 2) * 128
            sg = Wg[:, c0:c0 + 256].rearrange("(j p) f -> p j f", p=128)
            su = Wu[:, c0:c0 + 256].rearrange("(j p) f -> p j f", p=128)
            S.dma('pool', lambda h, o=wgs, i=sg: h.dma_start(out=o, in_=i), writes=[('wg', slot)])
            S.dma('pool', lambda h, o=wus, i=su: h.dma_start(out=o, in_=i), writes=[('wu', slot)])
            if pr == 0:
                for ci in range(GS):
                    ws = (g % 2) * GS + ci
                    r0 = (g * GS + ci) * 128
                    S.dma('pool', lambda h, o=wdr[ws], r0=r0: h.dma_start(out=o, in_=Wd[r0:r0 + 128, :]),
                          writes=[('wd', ws)])
            for cc in range(2):
                ci = pr * 2 + cc
                for hf in range(2):
                    ts = slice(hf * 512, (hf + 1) * 512)
                    pg = ps_bank(k, hf * 2)
                    pu = ps_bank(k, hf * 2 + 1)
                    for j in range(NJ):
                        S.op('pe', lambda h, pg=pg, wgs=wgs, j=j, cc=cc, ts=ts: h.matmul(
                            pg, wgs[:, j, cc * 128:(cc + 1) * 128], k.hT[:, j, ts],
                            start=(j == 0), stop=(j == NJ - 1)),
                            reads=[('wg', slot), ('h', j, hf)], writes=[('psb', hf * 2)])
                    for j in range(NJ):
                        S.op('pe', lambda h, pu=pu, wus=wus, j=j, cc=cc, ts=ts: h.matmul(
                            pu, wus[:, j, cc * 128:(cc + 1) * 128], k.hT[:, j, ts],
                            start=(j == 0), stop=(j == NJ - 1)),
                            reads=[('wu', slot), ('h', j, hf)], writes=[('psb', hf * 2 + 1)])
                    sl = sil[hf]
                    S.op('act', lambda h, sl=sl, pg=pg: h.activation(out=sl, in_=pg, func=AF.Silu),
                         reads=[('psb', hf * 2)], writes=[('sil', hf)])
                    S.op('dve', lambda h, sl=sl, pu=pu, ci=ci, ts=ts, ab=ab: h.tensor_tensor(
                        out=ab[:, ci, ts], in0=pu, in1=sl, op=ALU.mult),
                        reads=[('psb', hf * 2 + 1), ('sil', hf)], writes=[('act', g % 2, ci, hf)])

    def down(g):
        ab = act[g % 2]
        for j in range(NJ):
            for hf in range(2):
                ts = slice(hf * 512, (hf + 1) * 512)
                b = 4 + st['evac'] % 4
                st['evac'] += 1
                pd = ps_bank(k, b)
                for ci in range(GS):
                    ws = (g % 2) * GS + ci
                    S.op('pe', lambda h, pd=pd, ws=ws, j=j, ci=ci, ts=ts, ab=ab: h.matmul(
                        pd, wdr[ws][:, j * 128:(j + 1) * 128], ab[:, ci, ts],
                        start=(ci == 0), stop=(ci == GS - 1)),
                        reads=[('wd', ws), ('act', g % 2, ci, hf)], writes=[('psb', b)])
                S.op('dve', lambda h, pd=pd, j=j, ts=ts: h.scalar_tensor_tensor(
                    out=k.xT[:, j, ts], in0=pd, scalar=k.colG[:, j:j + 1], in1=k.xT[:, j, ts],
                    op0=ALU.mult, op1=ALU.add),
                    reads=[('psb', b), 'colG', ('x', j, hf)], writes=[('x', j, hf)])

    for g in range(NG):
        gateup(g)
        if g > 0:
            down(g - 1)
    down(NG - 1)


def norm_stats(k, C):
    S = k.S
    sq = [C.bf16(512) for _ in range(2)]
    rstd = [C.f32(512) for _ in range(2)]
    for hf in range(2):
        ts = slice(hf * 512, (hf + 1) * 512)
        psq = ps_bank(k, 4 + hf)
        for j in range(NJ):
            q = sq[j % 2]
            S.op('act', lambda h, q=q, j=j, ts=ts: h.activation(out=q, in_=k.xT[:, j, ts], func=AF.Square),
                 reads=[('x', j, hf)], writes=[('sq', j % 2)])
            S.op('pe', lambda h, q=q, j=j, psq=psq: h.matmul(psq, k.ones16[:], q, start=(j == 0), stop=(j == NJ - 1)),
                 reads=[('sq', j % 2), 'ones16'], writes=[('psb', 4 + hf)])
        r = rstd[hf]
        S.op('act', lambda h, r=r, psq=psq: h.activation(out=r, in_=psq, func=AF.Sqrt, bias=k.epsc[:, 0:1], scale=1.0),
             reads=[('psb', 4 + hf), 'epsc'], writes=[('rstd', hf)])
        S.op('dve', lambda h, r=r: h.reciprocal(out=r, in_=r), reads=[('rstd', hf)], writes=[('rstd', hf)])
    return rstd


def mod_cols(k, l, s):
    S = k.S
    base = l * NMODC + s * 3 * NJ
    sh = k.modT[:, base:base + NJ]
    sc = k.modT[:, base + NJ:base + 2 * NJ]
    gate = k.modT[:, base + 2 * NJ:base + 3 * NJ]
    gcol = k.gT[:, (l * 3 + s) * NJ:(l * 3 + s + 1) * NJ]
    S.op('dve', lambda h: h.tensor_scalar(out=k.colA[:], in0=sc, scalar1=1.0, scalar2=float(np.sqrt(D)),
                                          op0=ALU.add, op1=ALU.mult), reads=['modT'], writes=['colA'])
    S.op('dve', lambda h: h.tensor_tensor(out=k.colA[:], in0=k.colA[:], in1=gcol, op=ALU.mult),
         reads=['colA', 'gT0', 'gT1'], writes=['colA'])
    return sh, gate


def pool_A(k, l):
    S = k.S
    S.barrier()
    C = Carver(k.R, k.RN)
    sh, gate = mod_cols(k, l, 1)
    rstd = norm_stats(k, C)
    HB = 16
    halo_src = C.f32(NJ * HB).rearrange("p (j t) -> p j t", j=NJ)
    t16 = C.f32(HB)
    for j in range(NJ):
        S.op('dve', lambda h, j=j: h.tensor_tensor(out=t16, in0=k.xT[:, j, TC - HB:TC], in1=rstd[1][:, 512 - HB:512],
                                                  op=ALU.mult),
             reads=[('x', j, 1), ('rstd', 1)], writes=['t16'])
        S.op('act', lambda h, j=j: h.activation(out=halo_src[:, j, :], in_=t16, func=AF.Identity,
                                                bias=sh[:, j:j + 1], scale=k.colA[:, j:j + 1]),
             reads=['t16', 'colA', 'modT'], writes=['halo_src'])
    hl = k.ex_w('phalo', NJ * HB)
    S.dma('pool', lambda h: h.dma_start(out=hl, in_=halo_src.rearrange("p j t -> p (j t)")),
          reads=['halo_src'], writes=['exo'])
    k.exchange('phalo', NJ * HB, ['exo'], 'halo_all_d')
    S.barrier()


def pool_B(k, l):
    S = k.S
    S.barrier()
    C = Carver(k.R, k.RN)
    sh, gate = mod_cols(k, l, 1)
    S.op('dve', lambda h: h.tensor_tensor(out=k.colG[:], in0=gate, in1=k.psT[:], op=ALU.mult),
         reads=['modT', 'psT'], writes=['colG'])
    rstd = norm_stats(k, C)
    HB = 16
    halo_all = C.f32(NCORES * NJ * HB).rearrange("p (r c) -> p r c", r=NCORES)
    halo = C.f32(NJ * HB).rearrange("p (j t) -> p j t", j=NJ)
    hallg = k.ex_all('phalo', NJ * HB)
    S.dma('pool', lambda h: h.dma_start(out=halo_all, in_=hallg.rearrange("(r p) c -> p r c", p=128)),
          reads=['halo_all_d'], writes=['halo_all'])
    hflat = halo.rearrange("p j t -> p (j t)")
    S.op('dve', lambda h: h.tensor_scalar(out=hflat, in0=halo_all[:, 0, :], scalar1=k.onehot[:, 0:1], scalar2=None,
                                          op0=ALU.mult), reads=['halo_all', 'onehot'], writes=['halo'])
    for r in range(1, NCORES):
        S.op('dve', lambda h, r=r: h.scalar_tensor_tensor(out=hflat, in0=halo_all[:, r, :], scalar=k.onehot[:, r:r + 1],
                                                         in1=hflat, op0=ALU.mult, op1=ALU.add),
             reads=['halo_all', 'onehot', 'halo'], writes=['halo'])
    wp = [C.bf16(4 * 512).rearrange("p (i o) -> p i o", i=4) for _ in range(4)]
    for g in range(4):
        S.dma('pool', lambda h, g=g: h.dma_start(out=wp[g], in_=k.pool_w[g].rearrange("(i p) o -> p i o", p=128)),
              writes=[('wp', g)])
    W = HB + 512
    hb = [C.f32(W) for _ in range(2)]
    pp = [C.f32(W) for _ in range(2)]
    qq = [C.f32(W) for _ in range(2)]
    tmp = [C.f32(512) for _ in range(2)]
    fix = C.f32(HB)
    it = 0
    for j in range(NJ):
        L = 1 + j // 4
        w = 2 ** L
        for hf in (1, 0):
            ts = slice(hf * 512, (hf + 1) * 512)
            b = hb[it % 2]
            p_ = pp[it % 2]
            q_ = qq[it % 2]
            t = tmp[it % 2]
            sfx = it % 2
            it += 1
            S.op('dve', lambda h, t=t, j=j, ts=ts, hf=hf: h.tensor_tensor(out=t, in0=k.xT[:, j, ts], in1=rstd[hf], op=ALU.mult),
                 reads=[('x', j, hf), ('rstd', hf)], writes=[('ptmp', sfx)])
            S.op('act', lambda h, t=t, j=j, b=b: h.activation(out=b[:, HB:W], in_=t, func=AF.Identity,
                                                             bias=sh[:, j:j + 1], scale=k.colA[:, j:j + 1]),
                 reads=[('ptmp', sfx), 'colA', 'modT'], writes=[('hb', sfx)])
            if hf == 1:
                S.op('dve', lambda h, t=t, j=j: h.tensor_tensor(out=t[:, 0:HB], in0=k.xT[:, j, 512 - HB:512],
                                                               in1=rstd[0][:, 512 - HB:512], op=ALU.mult),
                     reads=[('x', j, 0), ('rstd', 0), ('ptmp', sfx)], writes=[('ptmp', sfx)])
                S.op('act', lambda h, t=t, j=j, b=b: h.activation(out=b[:, 0:HB], in_=t[:, 0:HB], func=AF.Identity,
                                                                 bias=sh[:, j:j + 1], scale=k.colA[:, j:j + 1]),
                     reads=[('ptmp', sfx), 'colA', 'modT'], writes=[('hb', sfx)])
            else:
                S.op('act', lambda h, j=j, b=b: h.copy(out=b[:, 0:HB], in_=halo[:, j, :]),
                     reads=['halo'], writes=[('hb', sfx)])
            src = b
            bufs = [p_, q_]
            for lev in range(L):
                sft = 2 ** lev
                dst = bufs[lev % 2]
                S.op('dve', lambda h, src=src, dst=dst, sft=sft: h.tensor_tensor(
                    out=dst[:, sft:W], in0=src[:, sft:W], in1=src[:, 0:W - sft], op=ALU.add),
                    reads=[('hb', sfx), ('pq', sfx)], writes=[('pq', sfx)])
                src = dst
            S.op('dve', lambda h, src=src, b=b, j=j, ts=ts, w=w: h.scalar_tensor_tensor(
                out=k.hT[:, j, ts], in0=src[:, HB:W], scalar=1.0 / w, in1=b[:, HB:W],
                op0=ALU.mult, op1=ALU.subtract),
                reads=[('pq', sfx), ('hb', sfx)], writes=[('h', j, hf)])
            if hf == 0:
                S.op('dve', lambda h, src=src, L=L: h.tensor_tensor(out=fix, in0=src[:, HB:2 * HB],
                                                                   in1=k.pinvc[:, (L - 1) * HB:L * HB], op=ALU.mult),
                     reads=[('pq', sfx), 'pinvc'], writes=['fix'])
                S.op('dve', lambda h, b=b, j=j: h.tensor_tensor(out=k.hT[:, j, 0:HB], in0=fix, in1=b[:, HB:2 * HB],
                                                               op=ALU.subtract),
                     reads=['fix', ('hb', sfx), ('h', j, 0)], writes=[('h', j, 0)])
    ev = 0
    for g in range(4):
        for oc in range(4):
            j = g * 4 + oc
            for hf in range(2):
                ts = slice(hf * 512, (hf + 1) * 512)
                bnk = ev % 4
                ev += 1
                pd = ps_bank(k, bnk)
                for ic in range(4):
                    S.op('pe', lambda h, pd=pd, g=g, ic=ic, oc=oc, ts=ts: h.matmul(
                        pd, wp[g][:, ic, oc * 128:(oc + 1) * 128], k.hT[:, g * 4 + ic, ts],
                        start=(ic == 0), stop=(ic == 3)),
                        reads=[('wp', g), ('h', g * 4 + ic, hf)], writes=[('psb', bnk)])
                S.op('dve', lambda h, pd=pd, j=j, ts=ts: h.scalar_tensor_tensor(
                    out=k.xT[:, j, ts], in0=pd, scalar=k.colG[:, j:j + 1], in1=k.xT[:, j, ts],
                    op0=ALU.mult, op1=ALU.add),
                    reads=[('psb', bnk), 'colG', ('x', j, hf)], writes=[('x', j, hf)])
    S.barrier()


def norm_to_hT(k, l, s, C):
    S = k.S
    sh, gate = mod_cols(k, l, s)
    C = Carver(k.R, k.RN)
    C.reset(k.RN - 3072)
    rstd = norm_stats(k, C)
    tmp = [C.f32(512) for _ in range(3)]
    for hf in range(2):
        ts = slice(hf * 512, (hf + 1) * 512)
        for j in range(NJ):
            t = tmp[j % 3]
            S.op('dve', lambda h, t=t, j=j, ts=ts, hf=hf: h.tensor_tensor(out=t, in0=k.xT[:, j, ts], in1=rstd[hf], op=ALU.mult),
                 reads=[('x', j, hf), ('rstd', hf)], writes=[('ntmp', j % 3)])
            S.op('act', lambda h, t=t, j=j, ts=ts: h.activation(out=k.hT[:, j, ts], in_=t, func=AF.Identity,
                                                          bias=sh[:, j:j + 1], scale=k.colA[:, j:j + 1]),
                 reads=[('ntmp', j % 3), 'colA', 'modT'], writes=[('h', j, hf)])
    return gate


def swa_layout(k):
    C = Carver(k.R, k.RN)
    NT = TC // 128
    L = {}
    L['QT'] = C.bf16(NJ * TC).rearrange("p (c t) -> p c t", c=NJ)
    L['KT'] = C.bf16(4 * (128 + TC)).rearrange("p (v t) -> p v t", v=4)
    L['VA'] = C.bf16((NT + 1) * 4 * 65).rearrange("p (n v e) -> p n v e", n=NT + 1, v=4)
    L['tab'] = C.f32(96)
    L['esink'] = C.f32(32)
    L['slq'] = C.bf16(32 * 128).rearrange("p (a q) -> p a q", a=32)
    L['cm'] = C.bf16(3 * 128).rearrange("p (a q) -> p a q", a=3)
    HW = 256 + 130
    L['hsrc'] = C.f32(HW)
    return C, L


def swa_A(k, l):
    S = k.S
    S.barrier()
    Wqkv = k.swa_qkv
    NT = TC // 128
    gate = norm_to_hT(k, l, 1, None)
    S.op('dve', lambda h: h.tensor_copy(out=k.colG[:], in_=gate), reads=['modT'], writes=['colG'])
    C, L = swa_layout(k)
    QT, KT, VA, tab, esink, slq, cm, hsrc = (L[x] for x in ('QT', 'KT', 'VA', 'tab', 'esink', 'slq', 'cm', 'hsrc'))
    S.dma('sp', lambda h: h.dma_start(out=tab, in_=k.swa_tab), writes=['swtab'])
    S.dma('sp', lambda h: h.dma_start(out=slq[0:3, :, :], in_=k.swa_slq), writes=['slq'])
    S.dma('sp', lambda h: h.dma_start(out=cm, in_=k.swa_cm), writes=['cm'])
    S.op('act', lambda h: h.activation(out=esink, in_=tab[:, 64:96], func=AF.Exp), reads=['swtab'], writes=['esink'])
    S.op('dve', lambda h: h.memset(VA[:, :, :, 64:65], 1.0), writes=['VAones'])
    pmark = C.mark()
    wring = [C.bf16(NJ * 128).rearrange("p (j f) -> p j f", j=NJ) for _ in range(4)]
    wst = {'n': 0}

    def wslot():
        i = wst['n'] % 4
        wst['n'] += 1
        return wring[i], i
    ev = 0
    for v in range(4):
        wk, ws = wslot()
        src = Wqkv[:, D + v * 64:D + (v + 1) * 64].rearrange("(j p) f -> p j f", p=128)
        for e in range(2):
            S.dma('pool', lambda h, wk=wk, e=e, src=src: h.dma_start(out=wk[:, :, e * 64:(e + 1) * 64], in_=src),
                  writes=[('wring', ws, e)])
        for hf in range(2):
            b = ev % 4
            ev += 1
            pk = ps_bank(k, b)
            for j in range(NJ):
                S.op('pe', lambda h, pk=pk, wk=wk, j=j, hf=hf: h.matmul(
                    pk, wk[:, j, :], k.hT[:, j, hf * 512:(hf + 1) * 512], start=(j == 0), stop=(j == NJ - 1)),
                    reads=[('wring', ws, 0), ('wring', ws, 1), ('h', j, hf)], writes=[('psb', b)])
            S.op('act', lambda h, pk=pk, v=v, hf=hf: h.copy(out=KT[:, v, 128 + hf * 512:128 + (hf + 1) * 512], in_=pk),
                 reads=[('psb', b)], writes=[('KT', v, hf)])
    wv, wvs = wslot()
    for e in range(2):
        S.dma('pool', lambda h, e=e: h.dma_start(
            out=wv[:, :, e * 64:(e + 1) * 64],
            in_=Wqkv[:, D + 256:D + 512].rearrange("(j p) f -> p j f", p=128)[:, :, e * 64:(e + 1) * 64]),
            writes=[('wring', wvs, e)])
    wv2, wvs2 = wslot()
    for e in range(2):
        S.dma('pool', lambda h, e=e: h.dma_start(
            out=wv2[:, :, e * 64:(e + 1) * 64],
            in_=Wqkv[:, D + 256:D + 512].rearrange("(j p) f -> p j f", p=128)[:, :, 128 + e * 64:128 + (e + 1) * 64]),
            writes=[('wring', wvs2, e)])
    for tt in range(NT):
        b = ev % 4
        ev += 1
        pv = ps_bank(k, b, 256)
        for vh, (wvx, wsx) in enumerate([(wv, wvs), (wv2, wvs2)]):
            for j in range(NJ):
                S.op('pe', lambda h, pv=pv, tt=tt, j=j, wvx=wvx, vh=vh: h.matmul(
                    pv[:, vh * 128:(vh + 1) * 128], k.hT[:, j, tt * 128:(tt + 1) * 128], wvx[:, j, :],
                    start=(j == 0), stop=(j == NJ - 1)),
                    reads=[('wring', wsx, 0), ('wring', wsx, 1), ('h', j, tt // 4)], writes=[('psb', b)])
        S.op('dve', lambda h, pv=pv, tt=tt: h.tensor_copy(out=VA[:, tt + 1, :, 0:64],
                                                          in_=pv.rearrange("p (v e) -> p v e", v=4)),
             reads=[('psb', b), 'VAones'], writes=[('VA', tt + 1)])
    for qc in range(NJ):
        w, ws = wslot()
        S.dma('pool', lambda h, w=w, qc=qc: h.dma_start(
            out=w, in_=Wqkv[:, qc * 128:(qc + 1) * 128].rearrange("(j p) f -> p j f", p=128)),
            writes=[('wring', ws, 0), ('wring', ws, 1)])
        if True:
            for hf in range(2):
                b = ev % 4
                ev += 1
                pq = ps_bank(k, b)
                for j in range(NJ):
                    S.op('pe', lambda h, pq=pq, w=w, j=j, hf=hf: h.matmul(
                        pq, w[:, j, :], k.hT[:, j, hf * 512:(hf + 1) * 512],
                        start=(j == 0), stop=(j == NJ - 1)),
                        reads=[('wring', ws, 0), ('wring', ws, 1), ('h', j, hf)], writes=[('psb', b)])
                if (qc + hf) % 2 == 0:
                    S.op('act', lambda h, pq=pq, qc=qc, hf=hf: h.copy(out=QT[:, qc, hf * 512:(hf + 1) * 512], in_=pq),
                         reads=[('psb', b)], writes=[('QT', qc, hf)])
                else:
                    S.op('dve', lambda h, pq=pq, qc=qc, hf=hf: h.tensor_copy(out=QT[:, qc, hf * 512:(hf + 1) * 512], in_=pq),
                         reads=[('psb', b)], writes=[('QT', qc, hf)])
    HW = 256 + 130
    hsrc16 = hsrc.bitcast(BF16)
    S.op('dve', lambda h: h.tensor_copy(out=hsrc16[:, 0:512].rearrange("p (v t) -> p v t", v=4), in_=KT[:, :, TC:TC + 128]),
         reads=[('KT', v, 1) for v in range(4)], writes=['hsrcK'])
    S.op('dve', lambda h: h.tensor_copy(out=hsrc16[:, 512:772], in_=VA[:, NT, :, :].rearrange("p v e -> p (v e)")),
         reads=[('VA', NT), 'VAones'], writes=['hsrcV'])
    shl = k.ex_w('shalo', HW)
    S.dma('pool', lambda h: h.dma_start(out=shl, in_=hsrc), reads=['hsrcK', 'hsrcV'], writes=['exo'])
    k.exchange('shalo', HW, ['exo'], 'swa_ha_d')
    S.barrier()


def swa_B(k, l):
    S = k.S
    S.barrier()
    Wo = k.swa_o
    NT = TC // 128
    SC = 0.125
    HW = 256 + 130
    C, L = swa_layout(k)
    QT, KT, VA, tab, esink, slq, cm, hsrc = (L[x] for x in ('QT', 'KT', 'VA', 'tab', 'esink', 'slq', 'cm', 'hsrc'))
    hall = C.f32(NCORES * HW).rearrange("p (r c) -> p r c", r=NCORES)
    hsel = C.f32(HW)
    shall = k.ex_all('shalo', HW)
    S.dma('pool', lambda h: h.dma_start(out=hall, in_=shall.rearrange("(r p) c -> p r c", p=128)),
          reads=['swa_ha_d'], writes=['hall'])
    hall16 = [hall[:, r, :].bitcast(BF16) for r in range(NCORES)]
    hsel16 = hsel.bitcast(BF16)
    S.op('dve', lambda h: h.tensor_scalar(out=hsel16, in0=hall16[0], scalar1=k.onehot[:, 0:1], scalar2=None, op0=ALU.mult),
         reads=['hall', 'onehot'], writes=['hsel'])
    for r in range(1, NCORES):
        S.op('dve', lambda h, r=r: h.scalar_tensor_tensor(out=hsel16, in0=hall16[r], scalar=k.onehot[:, r:r + 1], in1=hsel16,
                                                         op0=ALU.mult, op1=ALU.add),
             reads=['hall', 'onehot', 'hsel'], writes=['hsel'])
    S.op('dve', lambda h: h.tensor_copy(out=KT[:, :, 0:128], in_=hsel16[:, 0:512].rearrange("p (v t) -> p v t", v=4)),
         reads=['hsel'], writes=['KThalo'])
    S.op('dve', lambda h: h.tensor_copy(out=VA[:, 0, :, :].rearrange("p v e -> p (v e)"), in_=hsel16[:, 512:772]),
         reads=['hsel', 'VAones'], writes=[('VA', 0)])
    PT = [C.bf16(256).rearrange("p (a q) -> p a q", a=2) for _ in range(3)]
    Ost = [C.bf16(D) for _ in range(2)]
    den = [C.f32(1) for _ in range(4)]
    PSB16 = [k.PS[:, 6 * 512:7 * 512].bitcast(BF16), k.PS[:, 7 * 512:8 * 512].bitcast(BF16)]
    it = 0
    for qt in range(NT):
        os_ = Ost[qt % 2]
        for hq in range(32):
            v = hq // 8
            e = hq % 2
            qc = hq // 2
            pr = slice(e * 64, (e + 1) * 64)
            b = (it % 2) * 2
            ob = 4 + (it % 2)
            pt = PT[it % 3]
            dn = den[it % 4]
            sfx = it
            it += 1
            ps = [ps_bank(k, b, 128), ps_bank(k, b + 1, 128)]
            po = ps_bank(k, ob, 65)
            for a in range(2):
                kcols = slice(qt * 128 + a * 128, qt * 128 + a * 128 + 128)
                mi = (2 if qt == 0 else 0) if a == 0 else 1
                rd = [('KT', v, 0), ('KT', v, 1), 'KThalo', ('QT', qc, qt // 4)]
                S.op('pe', lambda h, ps=ps, a=a, v=v, pr=pr, kcols=kcols, qc=qc, qt=qt: h.matmul(
                    ps[a], KT[pr, v, kcols], QT[pr, qc, qt * 128:(qt + 1) * 128], start=True, stop=False),
                    reads=rd, writes=[('psb', b + a)])
                S.op('pe', lambda h, ps=ps, a=a, mi=mi: h.matmul(
                    ps[a], k.ident16[:], cm[:, mi, :], start=False, stop=False),
                    reads=['ident16', 'cm'], writes=[('psb', b + a)])
                S.op('pe', lambda h, ps=ps, a=a, hq=hq: h.matmul(
                    ps[a], k.ones16[0:3, :], slq[0:3, hq, :], start=False, stop=True),
                    reads=['ones16', 'slq'], writes=[('psb', b + a)])
                S.op('act', lambda h, ps=ps, a=a, pt=pt, hq=hq: h.activation(
                    out=pt[:, a, :], in_=ps[a], func=AF.Exp, bias=tab[:, a * 32 + hq:a * 32 + hq + 1], scale=SC),
                    reads=[('psb', b + a), 'swtab'], writes=[('PT', it % 3, a)])
            for a in range(2):
                S.op('pe', lambda h, po=po, pt=pt, a=a, qt=qt, v=v: h.matmul(
                    po, pt[:, a, :], VA[:, qt + a, v, :], start=(a == 0), stop=(a == 1)),
                    reads=[('PT', it % 3, a), ('VA', qt + a), 'VAones'], writes=[('psb', ob)])
            S.op('dve', lambda h, dn=dn, po=po, hq=hq: h.tensor_tensor(out=dn, in0=po[:, 64:65], in1=esink[:, hq:hq + 1], op=ALU.add),
                 reads=[('psb', ob), 'esink'], writes=[('den', it % 4)])
            S.op('dve', lambda h, dn=dn: h.reciprocal(out=dn, in_=dn), reads=[('den', it % 4)], writes=[('den', it % 4)])
            S.op('dve', lambda h, dn=dn, po=po, hq=hq, os_=os_: h.tensor_scalar(
                out=os_[:, hq * 64:(hq + 1) * 64], in0=po[:, 0:64], scalar1=dn[:, 0:1], scalar2=None, op0=ALU.mult),
                reads=[('psb', ob), ('den', it % 4)], writes=[('Ost', qt % 2, hq // 2)])
        for qc in range(NJ):
            S.op('pe', lambda h, qc=qc, os_=os_: h.transpose(out=PSB16[qc % 2][:, 0:128],
                                                            in_=os_[:, qc * 128:(qc + 1) * 128], identity=k.ident16[:]),
                 reads=[('Ost', qt % 2, qc), 'ident16'], writes=[('psb', 6 + qc % 2)])
            S.op('act', lambda h, qc=qc, qt=qt: h.copy(out=k.hT[:, qc, qt * 128:(qt + 1) * 128],
                                                       in_=PSB16[qc % 2][:, 0:128]),
                 reads=[('psb', 6 + qc % 2)], writes=[('h', qc, qt // 4)])
    S.barrier()
    C2 = Carver(k.R, k.RN)
    proj_out(k, Wo, C2)
    S.barrier()


def proj_out(k, Wo, C):
    S = k.S
    NS = 4
    wo = [C.bf16(D) for _ in range(NS)]
    for jq in range(4):
        for r in range(NJ):
            slot = (jq * NJ + r) % NS
            S.dma('pool', lambda h, slot=slot, r=r, jq=jq: h.dma_start(
                out=wo[slot][:, 0:512], in_=Wo[r * 128:(r + 1) * 128, jq * 512:(jq + 1) * 512]),
                writes=[('wo', slot)])
            for jj in range(4):
                for hf in range(2):
                    b = jj * 2 + hf
                    S.op('pe', lambda h, b=b, slot=slot, jj=jj, r=r, hf=hf: h.matmul(
                        ps_bank(k, b), wo[slot][:, jj * 128:(jj + 1) * 128], k.hT[:, r, hf * 512:(hf + 1) * 512],
                        start=(r == 0), stop=(r == NJ - 1)),
                        reads=[('wo', slot), ('h', r, hf)], writes=[('psb', b)])
        for jj in range(4):
            j = jq * 4 + jj
            for hf in range(2):
                b = jj * 2 + hf
                ts = slice(hf * 512, (hf + 1) * 512)
                S.op('dve', lambda h, b=b, j=j, ts=ts: h.scalar_tensor_tensor(
                    out=k.xT[:, j, ts], in0=ps_bank(k, b), scalar=k.colG[:, j:j + 1], in1=k.xT[:, j, ts],
                    op0=ALU.mult, op1=ALU.add),
                    reads=[('psb', b), 'colG', ('x', j, hf)], writes=[('x', j, hf)])


KVW = 8192 + 8256 + 64
MSC = float(128 ** -0.5)


def moba_A(k, l, jm):
    S = k.S
    S.barrier()
    C = Carver(k.R, k.RN)
    Wqkv = k.mobaw('qkv', jm)
    NT = TC // 128
    gate = norm_to_hT(k, l, 1, C)
    S.op('dve', lambda h: h.tensor_copy(out=k.colG[:], in_=gate), reads=['modT'], writes=['colG'])
    QT = C.bf16(16 * TC).rearrange("p (c t) -> p c t", c=16)
    base = C.mark()
    wr = [C.bf16(NJ * 256).rearrange("p (j f) -> p j f", j=NJ) for _ in range(3)]
    kst32 = [C.f32(512) for _ in range(2)]
    vst32 = [C.f32(1032) for _ in range(2)]
    kms = C.f32(64)
    st = {'w': 0, 'ev': 0}
    kv_keys = []

    def load_w(c0):
        slot = st['w'] % 3
        st['w'] += 1
        w = wr[slot]
        S.dma('pool', lambda h, w=w, c0=c0: h.dma_start(
            out=w, in_=Wqkv[:, c0:c0 + 256].rearrange("(j p) f -> p j f", p=128)), writes=[('wr', slot)])
        return w, slot

    pending = []

    def flush_ex():
        while pending:
            h2 = pending.pop(0)
            k.exchange('kv%d' % h2, 1028, [('kvl', 'K', h2), ('kvl', 'V', h2)], ('kvall', h2))
    for vb in range(2):
        v16 = vst32[vb].bitcast(BF16).rearrange("p (h n e) -> p h n e", h=2, n=NT)
        S.op('dve', lambda h, v16=v16: h.memset(v16[:, :, :, 128:129], 1.0), writes=[('vones', vb)])
    for hp in range(8):
        w, slot = load_w(hp * 256)
        for cc in range(2):
            hh = hp * 2 + cc
            for hf in range(2):
                b = st['ev'] % 4
                st['ev'] += 1
                pq = ps_bank(k, b)
                for j in range(NJ):
                    S.op('pe', lambda h, pq=pq, w=w, cc=cc, j=j, hf=hf: h.matmul(
                        pq, w[:, j, cc * 128:(cc + 1) * 128], k.hT[:, j, hf * 512:(hf + 1) * 512],
                        start=(j == 0), stop=(j == NJ - 1)),
                        reads=[('wr', slot), ('h', j, hf)], writes=[('psb', b)])
                S.op('act', lambda h, pq=pq, hh=hh, hf=hf: h.copy(out=QT[:, hh, hf * 512:(hf + 1) * 512], in_=pq),
                     reads=[('psb', b)], writes=[('QT', hh, hf)])
        w, slot = load_w(D + hp * 256)
        for cc in range(2):
            hh = hp * 2 + cc
            kb = hh % 2
            k16 = kst32[kb].bitcast(BF16)
            for hf in range(2):
                b = st['ev'] % 4
                st['ev'] += 1
                pk = ps_bank(k, b)
                for j in range(NJ):
                    S.op('pe', lambda h, pk=pk, w=w, cc=cc, j=j, hf=hf: h.matmul(
                        pk, w[:, j, cc * 128:(cc + 1) * 128], k.hT[:, j, hf * 512:(hf + 1) * 512],
                        start=(j == 0), stop=(j == NJ - 1)),
                        reads=[('wr', slot), ('h', j, hf)], writes=[('psb', b)])
                S.op('act', lambda h, pk=pk, k16=k16, hf=hf: h.copy(out=k16[:, hf * 512:(hf + 1) * 512], in_=pk),
                     reads=[('psb', b)], writes=[('kst', kb, hf)])
                S.op('dve', lambda h, k16=k16, hh=hh, hf=hf: h.tensor_reduce(
                    out=kms[:, hh * 4 + hf * 2:hh * 4 + hf * 2 + 2],
                    in_=k16[:, hf * 512:(hf + 1) * 512].rearrange("p (b t) -> p b t", b=2),
                    axis=AX.X, op=ALU.add), reads=[('kst', kb, hf)], writes=['kms'])
            S.dma('sp', lambda h, kb=kb, hh=hh: h.dma_start(out=k.kv_w(hh)[0], in_=kst32[kb]),
                  reads=[('kst', kb, 0), ('kst', kb, 1)], writes=[('kvl', 'K', hh)])
            kv_keys.append(('kvl', 'K', hh))
        w, slot = load_w(2 * D + hp * 256)
        flush_ex()
        vb = hp % 2
        v16 = vst32[vb].bitcast(BF16).rearrange("p (h n e) -> p h n e", h=2, n=NT)
        for tt in range(NT):
            b = st['ev'] % 4
            st['ev'] += 1
            pv = ps_bank(k, b, 256)
            for j in range(NJ):
                S.op('pe', lambda h, pv=pv, w=w, j=j, tt=tt: h.matmul(
                    pv, k.hT[:, j, tt * 128:(tt + 1) * 128], w[:, j, :], start=(j == 0), stop=(j == NJ - 1)),
                    reads=[('wr', slot), ('h', j, tt // 4)], writes=[('psb', b)])
            S.op('dve', lambda h, pv=pv, v16=v16, tt=tt: h.tensor_copy(
                out=v16[:, :, tt, 0:128], in_=pv.rearrange("p (h e) -> p h e", h=2)),
                reads=[('psb', b), ('vones', vb)], writes=[('vst', vb)])
        for e2 in range(2):
            hh2 = hp * 2 + e2
            S.dma('sp', lambda h, vb=vb, hh2=hh2, e2=e2: h.dma_start(
                out=k.kv_w(hh2)[1], in_=vst32[vb][:, e2 * 516:(e2 + 1) * 516]),
                reads=[('vst', vb), ('vones', vb)], writes=[('kvl', 'V', hh2)])
            kv_keys.append(('kvl', 'V', hh2))
            pending.append(hh2)
    S.op('dve', lambda h: h.tensor_scalar(out=kms, in0=kms, scalar1=1.0 / 256.0, scalar2=None, op0=ALU.mult),
         reads=['kms'], writes=['kms'])
    flush_ex()
    S.dma('sp', lambda h: h.dma_start(out=k.km_w(), in_=kms), reads=['kms'], writes=[('kvl', 'M')])
    kv_keys.append(('kvl', 'M'))
    k.exchange('km', 64, [('kvl', 'M')], 'km_all')
    S.op('sp', lambda h: h.nop(), reads=kv_keys, writes=['exo'])
    S.barrier()


def moba_B(k, l, jm):
    S = k.S
    S.barrier()
    Wo = k.mobaw('o', jm)
    NT = TC // 128
    C = Carver(k.R, k.RN)
    QT = C.bf16(16 * TC).rearrange("p (c t) -> p c t", c=16)
    km32 = C.f32(NCORES * 64).rearrange("p (r c) -> p r c", r=NCORES)
    km16 = C.bf16(NCORES * 64).rearrange("p (r c) -> p r c", r=NCORES)
    pastb = C.f32(128).rearrange("p (a n) -> p a n", a=4)
    TL = C.bf16(32 * 128)
    cmask = C.bf16(512).rearrange("p (a q) -> p a q", a=2)
    bown = C.f32(32)
    TR = [C.bf16(1024).rearrange("p (a q) -> p a q", a=2) for _ in range(2)]
    btab = [C.f32(128) for _ in range(2)]
    Kc = [C.f32(1024) for _ in range(3)]
    Vc = [C.f32(1032) for _ in range(3)]
    KL = [C.f32(512) for _ in range(2)]
    VL = [C.f32(516) for _ in range(2)]
    PT = [C.bf16(512) for _ in range(3)]
    gm = [C.f32(32) for _ in range(2)]
    top8 = [C.f32(8) for _ in range(2)]
    thr = [C.f32(1) for _ in range(2)]
    selb = [C.bf16(32) for _ in range(2)]
    On = [C.bf16(128) for _ in range(2)]
    rden = [C.f32(1) for _ in range(4)]
    S.dma('sp', lambda h: h.dma_start(out=km32, in_=k.km_all()), reads=['km_all'], writes=['km32'])
    S.op('dve', lambda h: h.tensor_copy(out=km16, in_=km32), reads=['km32'], writes=['km16'])
    S.dma('sp', lambda h: h.dma_start(out=pastb, in_=k.moba_pastb.rearrange("p (a n) -> p a n", a=4)), writes=['pastb'])
    S.op('dve', lambda h: h.memset(TL, 0.0), writes=['TL'])
    for tr_ in TR:
        S.op('dve', lambda h, tr_=tr_: h.memset(tr_, 0.0), writes=[('TRz', id(tr_))])
    S.dma('sp', lambda h: h.dma_start(out=TL[0:35, :], in_=k.moba_TL), reads=['TL'], writes=['TL'])
    S.dma('sp', lambda h: h.dma_start(out=cmask, in_=k.moba_cmask), writes=['cmask'])
    S.dma('sp', lambda h: h.dma_start(out=bown, in_=k.moba_bown), writes=['bown'])
    G7 = k.PS[:, 7 * 512:7 * 512 + 128]
    T7 = k.PS[:, 7 * 512 + 128:8 * 512].bitcast(BF16)
    acc = {}
    for qt in range(NT):
        bnk = 3 + qt // 2
        off = (qt % 2) * 256
        acc[qt] = (k.PS[:, bnk * 512 + off:bnk * 512 + off + 129], bnk)
    cnt = {'s': 0, 'p': 0, 'kv': 0, 'g': 0, 't': 0, 'o': 0}
    pend_pv = []
    g2s = {}

    def prep_gate(hh, qt):
        gi = cnt['g'] % 4
        cnt['g'] += 1
        g2 = cnt['g'] % 2
        g2s[(hh, qt)] = g2
        pg = G7[:, gi * 32:(gi + 1) * 32]
        S.op('pe', lambda h, pg=pg, hh=hh, qt=qt: h.matmul(
            pg, QT[:, hh, qt * 128:(qt + 1) * 128], km16[:, :, hh * 4:(hh + 1) * 4], start=True, stop=True),
            reads=[('QT', hh, qt // 4), 'km16'], writes=[('psb', 7)])
        S.op('dve', lambda h, pg=pg, g2=g2, qt=qt: h.tensor_tensor(out=gm[g2], in0=pg, in1=pastb[:, qt // 2, :], op=ALU.add),
             reads=[('psb', 7), 'pastb'], writes=[('gm', g2)])
        S.op('dve', lambda h, g2=g2: h.max(out=top8[g2], in_=gm[g2]), reads=[('gm', g2)], writes=[('top8', g2)])
        S.op('dve', lambda h, g2=g2: h.tensor_scalar(out=thr[g2], in0=top8[g2][:, 2:3], scalar1=-1e29, scalar2=None, op0=ALU.max),
             reads=[('top8', g2)], writes=[('thr', g2)])
        S.op('dve', lambda h, g2=g2: h.tensor_scalar(out=gm[g2], in0=gm[g2], scalar1=thr[g2][:, 0:1], scalar2=1.0,
                                                     op0=ALU.is_ge, op1=ALU.subtract),
             reads=[('gm', g2), ('thr', g2)], writes=[('gm', g2)])
        S.op('dve', lambda h, g2=g2: h.tensor_scalar(out=selb[g2], in0=gm[g2], scalar1=BIG, scalar2=None, op0=ALU.mult),
             reads=[('gm', g2)], writes=[('selb', g2)])

    def prep_tr(hh, qt):
        g2 = g2s[(hh, qt)]
        sl_ = hh % 2
        tr_ = TR[sl_]
        ti = cnt['t'] % 6
        cnt['t'] += 1
        pt_ = T7[0:32, ti * 128:(ti + 1) * 128]
        S.op('pe', lambda h, pt_=pt_, g2=g2: h.transpose(out=pt_, in_=selb[g2], identity=k.ident16[:]),
             reads=[('selb', g2), 'ident16'], writes=[('psb', 7)])
        S.op('act', lambda h, pt_=pt_, tr_=tr_, qt=qt: h.copy(out=tr_[0:32, qt // 4, (qt % 4) * 128:(qt % 4 + 1) * 128], in_=pt_),
             reads=[('psb', 7)], writes=[('TR', sl_, qt // 4)])

    for hh in range(16):
        sl = hh % 2
        tr = TR[sl]
        bt = btab[sl]
        kl = KL[sl]
        vl = VL[sl]
        kl16 = kl.bitcast(BF16)
        vl16 = vl.bitcast(BF16)
        S.dma('sp', lambda h, tr=tr, hh=hh: h.dma_start(out=tr[32:35, :, :], in_=k.moba_slq[hh]), writes=[('TRs', sl)])
        S.dma('sp', lambda h, bt=bt, hh=hh: h.dma_start(out=bt, in_=k.moba_bias[hh]), writes=[('bt', sl)])
        S.dma('sp', lambda h, kl=kl, hh=hh: h.dma_start(out=kl, in_=k.kv_own(hh)[0]),
              reads=[('kvl', 'K', hh)], writes=[('KL', sl)])
        S.dma('sp', lambda h, vl=vl, hh=hh: h.dma_start(out=vl, in_=k.kv_own(hh)[1]),
              reads=[('kvl', 'V', hh)], writes=[('VL', sl)])
        if hh == 0:
            for qt in range(NT):
                prep_gate(0, qt)
                prep_tr(0, qt)
        for qb in range(4):
            for kt in range(2):
                sb = cnt['s'] % 3
                cnt['s'] += 1
                ps = ps_bank(k, sb, 256)
                pi = cnt['p'] % 3
                cnt['p'] += 1
                pt = PT[pi]
                ktile = 2 * qb + kt
                S.op('pe', lambda h, ps=ps, ktile=ktile, hh=hh, qb=qb, kl16=kl16: h.matmul(
                    ps, kl16[:, ktile * 128:(ktile + 1) * 128], QT[:, hh, qb * 256:(qb + 1) * 256], start=True, stop=False),
                    reads=[('KL', sl), ('QT', hh, qb // 2)], writes=[('psb', sb)])
                S.op('pe', lambda h, ps=ps, kt=kt: h.matmul(ps, k.ident16[:], cmask[:, kt, :], start=False, stop=False),
                     reads=['ident16', 'cmask'], writes=[('psb', sb)])
                S.op('pe', lambda h, ps=ps, tr=tr: h.matmul(ps, TL[32:35, 0:128], tr[32:35, 0, 0:256], start=False, stop=True),
                     reads=['TL', ('TRs', sl)], writes=[('psb', sb)])
                S.op('act', lambda h, ps=ps, pt=pt, hh=hh, kt=kt: h.activation(
                    out=pt[:, 0:256], in_=ps, func=AF.Exp, bias=bown[:, hh * 2 + kt:hh * 2 + kt + 1], scale=MSC),
                    reads=[('psb', sb), 'bown'], writes=[('PT', pi)])
                for i in range(2):
                    qt = 2 * qb + i
                    a_ap, a_b = acc[qt]
                    S.op('pe', lambda h, a_ap=a_ap, pt=pt, i=i, vl16=vl16, ktile=ktile, kt=kt: h.matmul(
                        a_ap, pt[:, i * 128:(i + 1) * 128], vl16[:, ktile * 129:(ktile + 1) * 129],
                        start=(kt == 0 and i == 0), stop=False),
                        reads=[('PT', pi), ('VL', sl)], writes=[('psb', a_b)])
        for ch in range(4):
            ks = cnt['kv'] % 3
            cnt['kv'] += 1
            kc = Kc[ks]
            vc = Vc[ks]
            S.dma('sp', lambda h, kc=kc, ch=ch, hh=hh: h.dma_start(
                out=kc.rearrange("p (r c) -> p r c", r=2), in_=k.kv_allh(hh)[0][:, 2 * ch:2 * ch + 2, :]),
                reads=[('kvall', hh)], writes=[('Kc', ks)])
            S.dma('sp', lambda h, vc=vc, ch=ch, hh=hh: h.dma_start(
                out=vc.rearrange("p (r c) -> p r c", r=2),
                in_=k.kv_allh(hh)[1][:, 2 * ch:2 * ch + 2, :]),
                reads=[('kvall', hh)], writes=[('Vc', ks)])
            kc16 = kc.bitcast(BF16)
            vc16 = vc.bitcast(BF16)
            for half in range(2):
                for mt in range(16):
                    m = ch * 16 + mt
                    n = m // 2
                    sb = cnt['s'] % 3
                    cnt['s'] += 1
                    ps = ps_bank(k, sb)
                    pi = cnt['p'] % 3
                    cnt['p'] += 1
                    pt = PT[pi]
                    S.op('pe', lambda h, ps=ps, kc16=kc16, mt=mt, hh=hh, half=half: h.matmul(
                        ps, kc16[:, mt * 128:(mt + 1) * 128], QT[:, hh, half * 512:(half + 1) * 512], start=True, stop=False),
                        reads=[('Kc', ks), ('QT', hh, half)], writes=[('psb', sb)])
                    S.op('pe', lambda h, ps=ps, n=n, tr=tr, half=half: h.matmul(
                        ps, TL[:, n * 128:(n + 1) * 128], tr[:, half, :], start=False, stop=True),
                        reads=['TL', ('TRs', sl), ('TR', sl, half), ('TRz', id(tr))], writes=[('psb', sb)])
                    S.op('act', lambda h, ps=ps, pt=pt, bt=bt, m=m, half=half: h.activation(
                        out=pt, in_=ps, func=AF.Exp, bias=bt[:, m * 2 + half:m * 2 + half + 1], scale=MSC),
                        reads=[('psb', sb), ('bt', sl)], writes=[('PT', pi)])
                    def emit_pv(pt=pt, pi=pi, vc16=vc16, mt=mt, m=m, half=half, ks=ks):
                        for i in range(4):
                            qt = half * 4 + i
                            a_ap, a_b = acc[qt]
                            S.op('pe', lambda h, a_ap=a_ap, pt=pt, i=i, vc16=vc16, mt=mt, m=m: h.matmul(
                                a_ap, pt[:, i * 128:(i + 1) * 128],
                                vc16[:, (mt // 8) * 1032 + (mt % 8) * 129:(mt // 8) * 1032 + (mt % 8) * 129 + 129],
                                start=False, stop=(m == 63)),
                                reads=[('PT', pi), ('Vc', ks)], writes=[('psb', a_b)])
                    if pend_pv:
                        pend_pv.pop(0)()
                    pend_pv.append(emit_pv)
                    tcn = ch * 32 + half * 16 + mt
                    if hh + 1 < 16 and tcn >= 10:
                        qn, rn = divmod(tcn - 10, 12)
                        if qn < NT and rn == 0:
                            prep_gate(hh + 1, qn)
                        elif qn < NT and rn == 6:
                            prep_tr(hh + 1, qn)
        while pend_pv:
            pend_pv.pop(0)()
        for qt in range(NT):
            a_ap, a_b = acc[qt]
            oi = cnt['o'] % 2
            ri = cnt['o'] % 4
            cnt['o'] += 1
            S.op('dve', lambda h, a_ap=a_ap, ri=ri: h.reciprocal(out=rden[ri], in_=a_ap[:, 128:129]),
                 reads=[('psb', a_b)], writes=[('rden', ri)])
            S.op('dve', lambda h, a_ap=a_ap, ri=ri, oi=oi: h.tensor_scalar(
                out=On[oi], in0=a_ap[:, 0:128], scalar1=rden[ri][:, 0:1], scalar2=None, op0=ALU.mult),
                reads=[('psb', a_b), ('rden', ri)], writes=[('On', oi)])
            ti = cnt['t'] % 6
            cnt['t'] += 1
            pt_ = T7[:, ti * 128:(ti + 1) * 128]
            S.op('pe', lambda h, pt_=pt_, oi=oi: h.transpose(out=pt_, in_=On[oi], identity=k.ident16[:]),
                 reads=[('On', oi), 'ident16'], writes=[('psb', 7)])
            S.op('act', lambda h, pt_=pt_, hh=hh, qt=qt: h.copy(out=k.hT[:, hh, qt * 128:(qt + 1) * 128], in_=pt_),
                 reads=[('psb', 7)], writes=[('h', hh, qt // 4)])
    S.barrier()
    C2 = Carver(k.R, k.RN)
    proj_out(k, Wo, C2)
    S.barrier()


def final_norm_out(k):
    S = k.S
    yo = k.dout("y", [TC, D])
    C = Carver(k.R, k.RN)
    sq = [C.bf16(512) for _ in range(2)]
    rstd = [C.f32(512) for _ in range(2)]
    yst = [C.f32(D) for _ in range(2)]
    S.op('dve', lambda h: h.tensor_scalar(out=k.colA[:], in0=k.fgT[:], scalar1=float(np.sqrt(D)), scalar2=None,
                                          op0=ALU.mult), reads=['fgT'], writes=['colA'])
    for hf in range(2):
        ts = slice(hf * 512, (hf + 1) * 512)
        psq = ps_bank(k, 4 + hf)
        for j in range(NJ):
            q = sq[j % 2]
            S.op('act', lambda h, q=q, j=j, ts=ts: h.activation(out=q, in_=k.xT[:, j, ts], func=AF.Square),
                 reads=[('x', j, hf)], writes=[('sq', j % 2)])
            S.op('pe', lambda h, q=q, j=j, psq=psq: h.matmul(psq, k.ones16[:], q, start=(j == 0), stop=(j == NJ - 1)),
                 reads=[('sq', j % 2), 'ones16'], writes=[('psb', 4 + hf)])
        r = rstd[hf]
        S.op('act', lambda h, r=r, psq=psq: h.activation(out=r, in_=psq, func=AF.Sqrt, bias=k.epsc[:, 0:1], scale=1.0),
             reads=[('psb', 4 + hf), 'epsc'], writes=[('rstd', hf)])
        S.op('dve', lambda h, r=r: h.reciprocal(out=r, in_=r), reads=[('rstd', hf)], writes=[('rstd', hf)])
        for j in range(NJ):
            S.op('dve', lambda h, j=j, r=r, ts=ts: h.scalar_tensor_tensor(
                out=k.xT[:, j, ts], in0=k.xT[:, j, ts], scalar=k.colA[:, j:j + 1], in1=r,
                op0=ALU.mult, op1=ALU.mult),
                reads=[('x', j, hf), ('rstd', hf), 'colA'], writes=[('x', j, hf)])
    for tt in range(TC // 128):
        ys = yst[tt % 2]
        hf = tt // 4
        for jg in range(NJ // 4):
            b = jg % 4
            pst = ps_bank(k, b).rearrange("p (a t) -> p a t", a=4)
            for a in range(4):
                j = jg * 4 + a
                S.op('pe', lambda h, j=j, a=a, pst=pst, tt=tt: h.transpose(
                    out=pst[:, a, :], in_=k.xT[:, j, tt * 128:(tt + 1) * 128], identity=k.ident32[:]),
                    reads=[('x', j, hf), 'ident32'], writes=[('psb', b)])
            dst = ys[:, jg * 512:(jg + 1) * 512]
            if jg % 2 == 0:
                S.op('dve', lambda h, dst=dst, b=b: h.tensor_copy(out=dst, in_=ps_bank(k, b)),
                     reads=[('psb', b)], writes=[('yst', tt % 2, jg)])
            else:
                S.op('act', lambda h, dst=dst, b=b: h.copy(out=dst, in_=ps_bank(k, b)),
                     reads=[('psb', b)], writes=[('yst', tt % 2, jg)])
        S.dma('sp', lambda h, ys=ys, tt=tt: h.dma_start(out=yo[tt * 128:(tt + 1) * 128, :], in_=ys),
              reads=[('yst', tt % 2, jg) for jg in range(4)], writes=[('out', tt)])


class HostData:
    def __init__(self, inp):
        f = lambda a: np.ascontiguousarray(np.asarray(a, dtype=np.float32))
        self.inp = inp
        self.f = f
        bf = ml_dtypes.bfloat16
        self.common = {
            'cvec': f(inp['c']).reshape(NJ, 128),
            'norm_g': f(inp['norm_g']).reshape(DEPTH * 3 * NJ, 128),
            'final_g': f(inp['final_g']).reshape(NJ, 128),
            'pool_scale': f(inp['pool_scale']).reshape(NJ, 128),
            'ident': np.eye(128, dtype=np.float32),
            'pool_w': f(inp['pool_w'])[0],
            'swa_w_qkv': f(inp['swa_w_qkv'])[0],
            'swa_w_o': f(inp['swa_w_o'])[0],
        }
        self.x = f(inp['x'])[0]
        self.ada_b = f(inp['ada_b']).reshape(DEPTH * NMODC, 128)
        sl32 = np.array([2.0 ** (-8.0 * (i + 1) / 32) for i in range(32)], np.float32)
        p = np.arange(128, dtype=np.float32)
        self.sl32, self.p = sl32, p

        def split3(v, axis):
            h_ = v.astype(bf); r_ = (v - h_.astype(np.float32)).astype(np.float32)
            m_ = r_.astype(bf); l_ = (r_ - m_.astype(np.float32)).astype(bf)
            return np.stack([h_, m_, l_], axis)
        vq = (-(sl32[:, None] * p[None, :]) / np.float32(0.125)).astype(np.float32)
        self.common['swa_slq'] = np.ascontiguousarray(split3(vq, 0))
        NEGM = np.float32(-240000.0)
        kk = np.arange(128)[:, None]; qq = np.arange(128)[None, :]
        self.cprev = np.where(kk > qq, 0.0, NEGM).astype(np.float32)
        self.cown = np.where(kk <= qq, 0.0, NEGM).astype(np.float32)
        self.NEGM = NEGM
        self.sinks = f(inp['swa_sinks'])[0]
        sl16 = np.array([2.0 ** (-8.0 * (i + 1) / 16) for i in range(16)], np.float32)
        self.sl16 = sl16
        q512 = np.arange(512, dtype=np.float32)
        vq16 = (-(sl16[:, None] * q512[None, :]) / np.float32(MSC)).astype(np.float32)
        sp3 = split3(vq16, 1)
        self.common['moba_slq'] = np.ascontiguousarray(np.stack([sp3, sp3], 2))
        TLm = np.zeros((35, 32, 128), np.float32)
        for n_ in range(32):
            TLm[n_, n_, :] = 1.0
        TLm[32:35] = 1.0
        self.common['moba_TL'] = np.ascontiguousarray(TLm.reshape(35, 4096).astype(bf))
        cmk = np.zeros((128, 2, 256), np.float32)
        for kt_ in range(2):
            cmk[:, kt_, :] = np.where((kt_ * 128 + p[:, None]) <= np.arange(256)[None, :], 0.0, -BIG)
        self.common['moba_cmask'] = np.ascontiguousarray(cmk.astype(bf))
        bown = np.zeros((128, 16, 2), np.float32)
        for kt_ in range(2):
            bown[:, :, kt_] = sl16[None, :] * (kt_ * 128 + p[:, None])
        self.common['moba_bown'] = np.ascontiguousarray(bown.reshape(128, 32))

    def get(self, name, c):
        inp, f, p = self.inp, self.f, self.p
        bf = ml_dtypes.bfloat16
        if name in self.common:
            return self.common[name]
        if name == 'x_c':
            return np.ascontiguousarray(self.x[c * TC:(c + 1) * TC])
        if name == 'ada_w_c':
            half = MODPC * 128
            return np.ascontiguousarray(np.asarray(inp['ada_w'][c // 2], dtype=np.float32)[:, (c % 2) * half:(c % 2 + 1) * half])
        if name == 'ada_b_c':
            return np.ascontiguousarray(self.ada_b[c * MODPC:(c + 1) * MODPC])
        if name[0] == 'w' and name[1] in 'gud' and name[2] == '_':
            _, l, s_ = name.split('_')
            key = {'g': 'ffn_w_gate', 'u': 'ffn_w_up', 'd': 'ffn_w_down'}[name[1]]
            return f(inp[key][int(l), int(s_)])
        if name.startswith('moba_qkv_'):
            return f(inp['moba_w_qkv'][int(name.split('_')[-1])])
        if name.startswith('moba_o_'):
            return f(inp['moba_w_o'][int(name.split('_')[-1])])
        if name == 'onehot_prev':
            oh = np.zeros((128, NCORES), np.float32)
            if c > 0:
                oh[:, c - 1] = 1.0
            return oh
        if name == 'pool_invc':
            inv = np.zeros((4, 16), np.float32)
            for L in range(4):
                w = 2 ** (L + 1)
                for t in range(16):
                    inv[L, t] = 1.0 / (min(t + 1, w) if c == 0 else w)
            return np.ascontiguousarray(np.broadcast_to(inv.reshape(1, 64), (128, 64)))
        if name == 'moba_pastb':
            pb = np.zeros((4, 32), np.float32)
            for qb_ in range(4):
                pb[qb_, :] = np.where(np.arange(32) < 4 * c + qb_, 0.0, -1e30)
            return np.ascontiguousarray(np.broadcast_to(pb.reshape(1, 128), (128, 128)))
        if name == 'moba_bias':
            mb = np.zeros((16, 128, 64, 2), np.float32)
            mm_ = np.arange(64, dtype=np.float32)
            for half_ in range(2):
                pos = 128.0 * mm_[None, :] + p[:, None] - (1024.0 * c + 512.0 * half_)
                mb[:, :, :, half_] = self.sl16[:, None, None] * pos[None, :, :]
            return np.ascontiguousarray(mb.reshape(16, 128, 128))
        if name == 'swa_tab':
            tab = np.zeros((128, 96), np.float32)
            tab[:, 0:32] = self.sl32[None, :] * (p[:, None] - 128.0)
            tab[:, 32:64] = self.sl32[None, :] * p[:, None]
            tab[:, 64:96] = self.sinks[None, :]
            return tab
        if name == 'swa_cm':
            c0 = self.cprev if c > 0 else np.full((128, 128), self.NEGM, np.float32)
            return np.ascontiguousarray(np.stack([self.cprev, self.cown, c0], 1).astype(bf))
        raise KeyError(name)


def run_segments(inputs, cfg_base, nseg=None):
    H = HostData(inputs)
    state = None
    ex = None
    res = None
    segs = range(len(SEGMENTS)) if nseg is None else range(nseg)
    for seg in segs:
        cfg = dict(cfg_base)
        cfg['seg'] = seg
        nc, k = build_program(cfg)
        maps = []
        ex_all = None
        if 'ex_all' in k.used_inputs:
            ex_all = np.ascontiguousarray(np.concatenate(ex, axis=0))
        for c in range(NCORES):
            m = {}
            for name in k.used_inputs:
                if name == 'state_in':
                    m[name] = state[c]
                elif name == 'ex_all':
                    m[name] = ex_all
                elif name == 'ex_own':
                    m[name] = ex[c]
                else:
                    m[name] = H.get(name, c)
            maps.append(m)
        res = run_bass_kernel_spmd(nc, maps, core_ids=list(range(NCORES))).results
        if 'state_out' in k.outputs:
            state = [np.asarray(r['state_out']) for r in res]
        if 'ex_out' in k.outputs:
            ex = [np.asarray(r['ex_out']) for r in res]
    return res


def run_fused(inputs, cfg_base):
    H = HostData(inputs)
    cfg = dict(cfg_base)
    cfg['fused'] = True
    nc, k = build_program(cfg)
    maps = [{name: H.get(name, c) for name in k.used_inputs} for c in range(NCORES)]
    return run_bass_kernel_spmd(nc, maps, core_ids=list(range(NCORES))).results


FUSED = False


def kernel(**inputs):
    res = run_fused(inputs, {}) if FUSED else run_segments(inputs, {})
    y = np.concatenate([np.asarray(r['y']) for r in res], axis=0)
    return y[None].astype(np.float32)
```
